# Optimizing a Trainium2 kernel written in Bass

```python
import math
import jax, jax.numpy as jnp
from jax import lax
import numpy as np

D_MODEL = 2048
BATCH = 4
SEQ = 4096
DEPTH = 2

PLE_DIM = 256
BRANCH_W = 1024
N_BRANCH = 3
CONV_W = BRANCH_W
CONV_K = 3
LRU_W = BRANCH_W
LRU_HEADS = 8
LRU_HD = LRU_W // LRU_HEADS
LRU_CONV_K = 4
LRU_C = 8.0
NSA_HEADS = 16
NSA_KV_HEADS = 4
NSA_HD = 64
NSA_W = NSA_HEADS * NSA_HD
KV_W = NSA_KV_HEADS * NSA_HD
N_NSA_BRANCH = 3
CMP_BLK = 32
CMP_STRIDE = 16
CMP_HIDDEN = 128
SLC_BLK = 64
SLC_TOPN = 16
WINDOW = 512
Q_CHUNK = 64
IN_COLS = 3 * CONV_W + 2 * LRU_W + NSA_W + 6 * KV_W + N_NSA_BRANCH * NSA_HEADS + N_BRANCH * D_MODEL
N_EXPERTS = 16
N_GROUPS = 4
EXPERTS_PER_GROUP = N_EXPERTS // N_GROUPS
TOP_K = 2
D_EXPERT = 1024
MOE_BLK = 256
ALPHA = (2 * DEPTH) ** 0.25
BETA = (8 * DEPTH) ** -0.25
LN_EPS = 1e-5
NEG = -1e30
FORCE = 1e9

kernel_name = 'hybrid_conv_lru_nsa_moe_deepnorm'


def layer_norm(x, g, b):
    x32 = x.astype(jnp.float32)
    mu = x32.mean(-1, keepdims=True)
    var = jnp.square(x32 - mu).mean(-1, keepdims=True)
    y = (x32 - mu) * lax.rsqrt(var + LN_EPS) * g.astype(jnp.float32) + b.astype(jnp.float32)
    return y.astype(x.dtype)


def causal_dwconv(u, w, b):
    k, c = w.shape
    y = lax.conv_general_dilated(u, w[:, None, :].astype(u.dtype), window_strides=(1,), padding=[(k - 1, 0)],
                                 dimension_numbers=('NWC', 'WIO', 'NWC'), feature_group_count=c)
    return y + b


def short_conv_mixer(u, gate_b, gate_c, w, b):
    return gate_b * causal_dwconv(gate_c * u, w, b)


def _lin_rec(c1, c2):
    a1, b1 = c1
    a2, b2 = c2
    return a1 * a2, a2 * b1 + b2


def rg_lru_mixer(gate_in, u, conv_w, conv_b, w_a, b_a, w_x, b_x, lam):
    bsz, seq, _ = u.shape
    xc = causal_dwconv(u, conv_w, conv_b)
    xh = xc.reshape(bsz, seq, LRU_HEADS, LRU_HD)
    r = jax.nn.sigmoid(jnp.einsum('bshi,hij->bshj', xh, w_a) + b_a).reshape(bsz, seq, LRU_W)
    i = jax.nn.sigmoid(jnp.einsum('bshi,hij->bshj', xh, w_x) + b_x).reshape(bsz, seq, LRU_W)
    log_a = -LRU_C * r.astype(jnp.float32) * jax.nn.softplus(-lam.astype(jnp.float32))
    a = jnp.exp(log_a)
    b_t = jnp.sqrt(-jnp.expm1(2.0 * log_a)) * (i * xc).astype(jnp.float32)
    _, h = lax.associative_scan(_lin_rec, (a, b_t), axis=1)
    return h.astype(u.dtype) * jax.nn.gelu(gate_in)


def nsa_compress(kv, pos, w1, b1, w2, blk_idx):
    bsz = kv.shape[0]
    nc = blk_idx.shape[0]
    blocks = kv[:, blk_idx] + pos[:, None, :]
    blocks = blocks.transpose(0, 1, 3, 2, 4).reshape(bsz, nc, NSA_KV_HEADS, CMP_BLK * NSA_HD)
    return jax.nn.gelu(blocks @ w1 + b1) @ w2


def nsa_mixer(q, kc_in, vc_in, ks_in, vs_in, kw_in, vw_in, gate_logits, cmp_pos, phi_w1, phi_b1, phi_w2):
    bsz, seq = q.shape[:2]
    dt = q.dtype
    grp = NSA_HEADS // NSA_KV_HEADS
    n_cmp = (seq - CMP_BLK) // CMP_STRIDE + 1
    n_slc = seq // SLC_BLK
    top_n = min(SLC_TOPN, n_slc)
    n_chunks = seq // Q_CHUNK
    c_start = np.arange(n_cmp) * CMP_STRIDE
    blk_idx = c_start[:, None] + np.arange(CMP_BLK)[None, :]
    cmp_end = jnp.asarray(c_start + CMP_BLK - 1, jnp.int32)
    s_start = np.arange(n_slc) * SLC_BLK
    overlap = np.clip(np.minimum(c_start[:, None] + CMP_BLK, s_start[None, :] + SLC_BLK)
                      - np.maximum(c_start[:, None], s_start[None, :]), 0, None)
    cmp_to_slc = jnp.asarray(overlap / CMP_BLK, jnp.float32)
    kc = nsa_compress(kc_in, cmp_pos[0], phi_w1[0], phi_b1[0], phi_w2[0], blk_idx)
    vc = nsa_compress(vc_in, cmp_pos[1], phi_w1[1], phi_b1[1], phi_w2[1], blk_idx)
    ks_blk = ks_in.reshape(bsz, n_slc, SLC_BLK, NSA_KV_HEADS, NSA_HD).transpose(0, 3, 1, 2, 4)
    vs_blk = vs_in.reshape(bsz, n_slc, SLC_BLK, NSA_KV_HEADS, NSA_HD).transpose(0, 3, 1, 2, 4)
    kw_pad = jnp.pad(kw_in, ((0, 0), (WINDOW, 0), (0, 0), (0, 0)))
    vw_pad = jnp.pad(vw_in, ((0, 0), (WINDOW, 0), (0, 0), (0, 0)))
    slopes = jnp.asarray(2.0 ** (-8.0 * np.arange(1, NSA_HEADS + 1) / NSA_HEADS), jnp.float32).reshape(NSA_KV_HEADS, grp)
    q = q * (NSA_HD ** -0.5)
    gates = jax.nn.sigmoid(gate_logits).reshape(bsz, seq, NSA_KV_HEADS, grp, N_NSA_BRANCH)
    b_idx = jnp.arange(bsz)[:, None, None, None]
    g_idx = jnp.arange(NSA_KV_HEADS)[None, :, None, None]
    slc_j = jnp.arange(n_slc)

    def attend_chunk(c):
        start = c * Q_CHUNK
        t = start + jnp.arange(Q_CHUNK)
        qc = lax.dynamic_slice_in_dim(q, start, Q_CHUNK, axis=1).reshape(bsz, Q_CHUNK, NSA_KV_HEADS, grp, NSA_HD)
        dist = (t[:, None] - cmp_end[None, :]).astype(jnp.float32)
        ok = dist >= 0
        s = jnp.einsum('bqgrd,bngd->bgrqn', qc, kc).astype(jnp.float32) - slopes[:, :, None, None] * dist
        p_cmp = jnp.where(ok, jax.nn.softmax(jnp.where(ok, s, NEG), axis=-1), 0.0)
        o_cmp = jnp.einsum('bgrqn,bngd->bqgrd', p_cmp.astype(dt), vc)
        imp = jnp.einsum('bgrqn,nj->bgqj', p_cmp, cmp_to_slc)
        forced = (slc_j[None, :] == 0) | (slc_j[None, :] == (t // SLC_BLK)[:, None])
        causal = slc_j[None, :] * SLC_BLK <= t[:, None]
        imp = jnp.where(forced, FORCE, jnp.where(causal, imp, -FORCE))
        _, idx = lax.top_k(imp, top_n)
        kb = ks_blk[b_idx, g_idx, idx]
        vb = vs_blk[b_idx, g_idx, idx]
        kpos = idx[..., None] * SLC_BLK + jnp.arange(SLC_BLK)
        dist = (t[None, None, :, None, None] - kpos).astype(jnp.float32)[:, :, None]
        s = jnp.einsum('bqgrd,bgqnsd->bgrqns', qc, kb).astype(jnp.float32) - slopes[None, :, :, None, None, None] * dist
        s = jnp.where(dist >= 0, s, NEG).reshape(bsz, NSA_KV_HEADS, grp, Q_CHUNK, top_n * SLC_BLK)
        p_slc = jax.nn.softmax(s, axis=-1).reshape(bsz, NSA_KV_HEADS, grp, Q_CHUNK, top_n, SLC_BLK)
        o_slc = jnp.einsum('bgrqns,bgqnsd->bqgrd', p_slc.astype(dt), vb)
        kw = lax.dynamic_slice_in_dim(kw_pad, start, WINDOW + Q_CHUNK, axis=1)
        vw = lax.dynamic_slice_in_dim(vw_pad, start, WINDOW + Q_CHUNK, axis=1)
        kpos_w = start - WINDOW + jnp.arange(WINDOW + Q_CHUNK)
        dist_w = t[:, None] - kpos_w[None, :]
        ok_w = (dist_w >= 0) & (dist_w < WINDOW) & (kpos_w[None, :] >= 0)
        s = jnp.einsum('bqgrd,bkgd->bgrqk', qc, kw).astype(jnp.float32) - slopes[:, :, None, None] * dist_w.astype(jnp.float32)
        p_win = jax.nn.softmax(jnp.where(ok_w, s, NEG), axis=-1)
        o_win = jnp.einsum('bgrqk,bkgd->bqgrd', p_win.astype(dt), vw)
        g = lax.dynamic_slice_in_dim(gates, start, Q_CHUNK, axis=1)
        o = g[..., 0:1] * o_cmp + g[..., 1:2] * o_slc + g[..., 2:3] * o_win
        return o.reshape(bsz, Q_CHUNK, NSA_W)

    out = lax.map(attend_chunk, jnp.arange(n_chunks))
    return out.transpose(1, 0, 2, 3).reshape(bsz, seq, NSA_W)


def token_mixer(x, w_in, conv_a_w, conv_a_b, lru_conv_w, lru_conv_b, lru_wa, lru_ba, lru_wx, lru_bx, lru_lam,
                cmp_pos, phi_w1, phi_b1, phi_w2, w_branch, w_out):
    bsz, seq, _ = x.shape
    widths = [CONV_W] * 3 + [LRU_W] * 2 + [NSA_W] + [KV_W] * 6 + [N_NSA_BRANCH * NSA_HEADS, N_BRANCH * D_MODEL]
    points = np.cumsum(widths)[:-1].tolist()
    z = x @ w_in
    (a_in, a_b, a_c, r_gate, r_in, q, kc, vc, ks, vs, kw, vw, nsa_g, merge_g) = jnp.split(z, points, axis=-1)
    y_a = short_conv_mixer(a_in, a_b, a_c, conv_a_w, conv_a_b)
    y_b = rg_lru_mixer(r_gate, r_in, lru_conv_w, lru_conv_b, lru_wa, lru_ba, lru_wx, lru_bx, lru_lam)
    kv_heads = lambda t: t.reshape(bsz, seq, NSA_KV_HEADS, NSA_HD)
    y_c = nsa_mixer(q.reshape(bsz, seq, NSA_HEADS, NSA_HD), kv_heads(kc), kv_heads(vc), kv_heads(ks), kv_heads(vs),
                    kv_heads(kw), kv_heads(vw), nsa_g.reshape(bsz, seq, NSA_HEADS, N_NSA_BRANCH),
                    cmp_pos, phi_w1, phi_b1, phi_w2)
    gl = jax.nn.sigmoid(merge_g).reshape(bsz, seq, N_BRANCH, D_MODEL)
    merged = (gl[:, :, 0] * (y_a @ w_branch[0]) + gl[:, :, 1] * (y_b @ w_branch[1])
              + gl[:, :, 2] * (y_c @ w_branch[2]))
    return merged @ w_out


def grouped_moe(x, w_router, b_router, w_gate_up, w_down):
    bsz, seq, d = x.shape
    n_tok = bsz * seq
    n_assign = n_tok * TOP_K
    xf = x.reshape(n_tok, d)
    affinity = jax.nn.sigmoid((xf @ w_router).astype(jnp.float32))
    select = affinity + b_router.astype(jnp.float32)
    group_score = lax.top_k(select.reshape(n_tok, N_GROUPS, EXPERTS_PER_GROUP), 2)[0].sum(-1)
    best_group = jnp.argmax(group_score, axis=-1)
    in_group = (jnp.arange(N_EXPERTS) // EXPERTS_PER_GROUP)[None, :] == best_group[:, None]
    _, expert_idx = lax.top_k(jnp.where(in_group, select, NEG), TOP_K)
    gate = jnp.take_along_axis(affinity, expert_idx, axis=-1)
    gate = gate / gate.sum(-1, keepdims=True)
    e_flat = expert_idx.reshape(n_assign)
    order = jnp.argsort(e_flat)
    e_sorted = e_flat[order]
    tok_sorted = (order // TOP_K).astype(jnp.int32)
    counts = jnp.bincount(e_flat, length=N_EXPERTS)
    padded = (counts + MOE_BLK - 1) // MOE_BLK * MOE_BLK
    pad_end = jnp.cumsum(padded)
    pad_start = pad_end - padded
    raw_start = jnp.cumsum(counts) - counts
    dest = pad_start[e_sorted] + jnp.arange(n_assign) - raw_start[e_sorted]
    n_rows = n_assign + N_EXPERTS * MOE_BLK
    n_blocks = n_rows // MOE_BLK
    row_tok = jnp.zeros((n_rows,), jnp.int32).at[dest].set(tok_sorted)
    block_expert = jnp.minimum(jnp.searchsorted(pad_end, jnp.arange(n_blocks) * MOE_BLK, side='right'), N_EXPERTS - 1)

    def expert_block(b):
        xb = xf[lax.dynamic_slice_in_dim(row_tok, b * MOE_BLK, MOE_BLK)]
        e = block_expert[b]
        g, u = jnp.split(xb @ w_gate_up[e], 2, axis=-1)
        return (jax.nn.silu(g) * u) @ w_down[e]

    rows = lax.map(expert_block, jnp.arange(n_blocks)).reshape(n_rows, d)
    dest_orig = jnp.zeros((n_assign,), dest.dtype).at[order].set(dest)
    y = rows[dest_orig].reshape(n_tok, TOP_K, d)
    return jnp.einsum('tk,tkd->td', gate.astype(x.dtype), y).reshape(bsz, seq, d)


def setup_inputs(seed: int = 0) -> dict:
    key = jax.random.key(seed)
    ks = jax.random.split(key, 26)
    f32 = jnp.float32

    def nrm(k, shape, scale):
        return jax.random.normal(k, shape, f32) * scale

    u = jax.random.uniform(ks[11], (DEPTH, LRU_W), f32, 0.9, 0.999)
    a0 = u ** (1.0 / LRU_C)
    return {
        'x': nrm(ks[0], (BATCH, SEQ, D_MODEL), 1.0),
        'p': nrm(ks[1], (DEPTH, BATCH, SEQ, PLE_DIM), 1.0),
        'w_in': nrm(ks[2], (DEPTH, D_MODEL, IN_COLS), D_MODEL ** -0.5),
        'conv_a_w': nrm(ks[3], (DEPTH, CONV_K, CONV_W), 0.5),
        'conv_a_b': nrm(ks[4], (DEPTH, CONV_W), 0.02),
        'lru_conv_w': nrm(ks[5], (DEPTH, LRU_CONV_K, LRU_W), 0.5),
        'lru_conv_b': nrm(ks[6], (DEPTH, LRU_W), 0.02),
        'lru_wa': nrm(ks[7], (DEPTH, LRU_HEADS, LRU_HD, LRU_HD), LRU_HD ** -0.5),
        'lru_ba': nrm(ks[8], (DEPTH, LRU_HEADS, LRU_HD), 0.02),
        'lru_wx': nrm(ks[9], (DEPTH, LRU_HEADS, LRU_HD, LRU_HD), LRU_HD ** -0.5),
        'lru_bx': nrm(ks[10], (DEPTH, LRU_HEADS, LRU_HD), 0.02),
        'lru_lam': jnp.log(a0) - jnp.log1p(-a0),
        'cmp_pos': nrm(ks[12], (DEPTH, 2, CMP_BLK, NSA_HD), 0.02),
        'phi_w1': nrm(ks[13], (DEPTH, 2, CMP_BLK * NSA_HD, CMP_HIDDEN), (CMP_BLK * NSA_HD) ** -0.5),
        'phi_b1': nrm(ks[14], (DEPTH, 2, CMP_HIDDEN), 0.02),
        'phi_w2': nrm(ks[15], (DEPTH, 2, CMP_HIDDEN, NSA_HD), CMP_HIDDEN ** -0.5),
        'w_branch': nrm(ks[16], (DEPTH, N_BRANCH, BRANCH_W, D_MODEL), BRANCH_W ** -0.5),
        'w_out': nrm(ks[17], (DEPTH, D_MODEL, D_MODEL), BETA * D_MODEL ** -0.5),
        'ln_g': 1.0 + nrm(ks[18], (DEPTH, 2, D_MODEL), 0.02),
        'ln_b': nrm(ks[19], (DEPTH, 2, D_MODEL), 0.02),
        'w_router': nrm(ks[20], (D_MODEL, N_EXPERTS), D_MODEL ** -0.5),
        'b_router': nrm(ks[21], (N_EXPERTS,), 0.01),
        'w_gate_up': nrm(ks[22], (DEPTH, N_EXPERTS, D_MODEL, 2 * D_EXPERT), D_MODEL ** -0.5),
        'w_down': nrm(ks[23], (DEPTH, N_EXPERTS, D_EXPERT, D_MODEL), BETA * D_EXPERT ** -0.5),
        'w_ple': nrm(ks[24], (DEPTH, PLE_DIM, D_MODEL), BETA * PLE_DIM ** -0.5),
        'w_ple_gate': nrm(ks[25], (DEPTH, D_MODEL, D_MODEL), D_MODEL ** -0.5),
    }


def reference(x, p, w_in, conv_a_w, conv_a_b, lru_conv_w, lru_conv_b, lru_wa, lru_ba, lru_wx, lru_bx, lru_lam,
              cmp_pos, phi_w1, phi_b1, phi_w2, w_branch, w_out, ln_g, ln_b, w_router, b_router,
              w_gate_up, w_down, w_ple, w_ple_gate):
    for i in range(DEPTH):
        mix = token_mixer(x, w_in[i], conv_a_w[i], conv_a_b[i], lru_conv_w[i], lru_conv_b[i], lru_wa[i], lru_ba[i],
                          lru_wx[i], lru_bx[i], lru_lam[i], cmp_pos[i], phi_w1[i], phi_b1[i], phi_w2[i],
                          w_branch[i], w_out[i])
        x = layer_norm(ALPHA * x + mix, ln_g[i, 0], ln_b[i, 0])
        ffn = grouped_moe(x, w_router, b_router, w_gate_up[i], w_down[i])
        ple = jax.nn.sigmoid(x @ w_ple_gate[i]) * (p[i] @ w_ple[i])
        x = layer_norm(ALPHA * x + ffn + ple, ln_g[i, 1], ln_b[i, 1])
    return x
```

```python
import numpy as np
from contextlib import ExitStack
import concourse.bass as bass
import concourse.mybir as mybir
from concourse.bass_utils import run_bass_kernel_spmd

F32 = mybir.dt.float32
BF16 = mybir.dt.bfloat16
AF = mybir.ActivationFunctionType
ALU = mybir.AluOpType
AX = mybir.AxisListType


class Buf:
    def __init__(self, name, t):
        self.name = name
        self.t = t
        self.w = []
        self.r = []
        self.r_old = []
        self.dsem = None

    def __getitem__(self, idx):
        return self.t[idx]


class KB:
    ENG = ("pe", "act", "dve", "pool", "sp")

    def __init__(self, nc, es):
        self.nc = nc
        self.es = es
        self.sem = {}
        self.count = {}
        self.known = {}
        for e in self.ENG:
            self.sem[e] = es.enter_context(nc.semaphore("sem_" + e))
            self.count[e] = 0
        self.dsem_pool = [es.enter_context(nc.semaphore("dsem%d" % i)) for i in range(80)]
        self.dsem_count = {}
        self.dsem_next = 0
        self.rec = {e: [] for e in self.ENG}
        self.known = {e: {} for e in self.ENG}
        self.stage_dsems = []
        self.uid = 0

    def sb(self, st, name, shape, dt):
        self.uid += 1
        t = st.enter_context(self.nc.sbuf_tensor("%s_%d" % (name, self.uid), list(shape), dt))
        return Buf(name, t)

    def ps(self, st, name, shape, dt):
        self.uid += 1
        t = st.enter_context(self.nc.psum_tensor("%s_%d" % (name, self.uid), list(shape), dt))
        return Buf(name, t)

    def _dsem(self, b):
        if b.dsem is None:
            s = self.dsem_pool[self.dsem_next % len(self.dsem_pool)]
            self.dsem_next += 1
            b.dsem = s
            self.dsem_count.setdefault(id(s), [s, 0])
        return b.dsem

    def _waits(self, eng, reads, writes, wadd):
        toks = []
        for b in reads:
            toks += b.w
        for b in writes:
            if not wadd:
                toks += b.w
            else:
                toks += b.r_old
            toks += b.r
        best = {}
        for (s, v, e) in toks:
            if eng == "pe" and e == "pe":
                continue
            k = id(s)
            if k not in best or best[k][1] < v:
                best[k] = (s, v)
        out = []
        kn = self.known[eng]
        for k, (s, v) in best.items():
            if kn.get(k, 0) >= v:
                continue
            kn[k] = v
            out.append((s, v))
        return out

    def op(self, eng, fn, reads=(), writes=(), wadd=False):
        waits = self._waits(eng, reads, writes, wadd)
        self.count[eng] += 1
        tok = (self.sem[eng], self.count[eng], eng)
        self.rec[eng].append((waits, fn, (self.sem[eng], 1)))
        for b in reads:
            b.r.append(tok)
        for b in writes:
            if wadd:
                b.w.append(tok)
            else:
                b.w = [tok]
                b.r_old = b.r
                b.r = []
        return tok

    def dma(self, q, out_ap, in_ap, reads=(), writes=(), wadd=False, dbuf=None, **kw):
        waits = self._waits(q, reads, writes, wadd)
        if dbuf is None:
            dbuf = writes[0] if writes else reads[0]
        s = self._dsem(dbuf)
        ent = self.dsem_count[id(s)]
        ent[1] += 16
        tok = (s, ent[1], "dma")

        def fn(e, out_ap=out_ap, in_ap=in_ap, kw=kw):
            try:
                return e.dma_start(out=out_ap, in_=in_ap, **kw)
            except Exception:
                print("DMA FAIL", out_ap, in_ap)
                raise

        self.rec[q].append((waits, fn, (s, 16)))
        for b in reads:
            b.r.append(tok)
        for b in writes:
            if wadd:
                b.w.append(tok)
            else:
                b.w = [tok]
                b.r_old = b.r
                b.r = []
        return tok

    def idma(self, out_ap, in_ap, idx_ap, scatter, bound, reads=(), writes=(), wadd=False, dbuf=None):
        waits = self._waits("pool", reads, writes, wadd)
        if dbuf is None:
            dbuf = writes[0]
        s = self._dsem(dbuf)
        ent = self.dsem_count[id(s)]
        ent[1] += 16
        tok = (s, ent[1], "dma")

        def fn(e):
            off = bass.IndirectOffsetOnAxis(ap=idx_ap, axis=0)
            if not hasattr(self, "bregs"):
                self.bregs = {}
            if bound not in self.bregs:
                r = e.alloc_register("bnd%d" % bound)
                e.reg_mov(r, bound)
                self.bregs[bound] = r
            breg = self.bregs[bound]
            if scatter:
                return e.indirect_dma_start(out=out_ap, out_offset=off, in_=in_ap, in_offset=None,
                                            bounds_check=breg, oob_is_err=False)
            return e.indirect_dma_start(out=out_ap, out_offset=None, in_=in_ap, in_offset=off,
                                        bounds_check=breg, oob_is_err=False)

        self.rec["pool"].append((waits, fn, (s, 16)))
        for b in reads:
            b.r.append(tok)
        for b in writes:
            if wadd:
                b.w.append(tok)
            else:
                b.w = [tok]
                b.r_old = b.r
                b.r = []
        return tok

    def flush(self, final=False):
        nc = self.nc
        tails = {}
        for e in self.ENG:
            w = []
            for f in self.ENG:
                if f != e and self.count[f] > self.known[e].get(id(self.sem[f]), 0):
                    w.append((self.sem[f], self.count[f]))
                    self.known[e][id(self.sem[f])] = self.count[f]
            for k, (s, v) in self.dsem_count.items():
                if v > self.known[e].get(k, 0):
                    w.append((s, v))
                    self.known[e][k] = v
            tails[e] = w
        rec = self.rec
        self.rec = {e: [] for e in self.ENG}
        assert self.dsem_next <= len(self.dsem_pool), self.dsem_next
        self.dsem_next = 0

        def replay(eng_obj, name):
            for (waits, fn, inc) in rec[name]:
                for (s, v) in waits:
                    eng_obj.wait_ge(s, v)
                ins = fn(eng_obj)
                ins.then_inc(inc[0], inc[1])
            for (s, v) in tails[name]:
                eng_obj.wait_ge(s, v)

        with nc.Block() as block:
            @block.tensor
            def _(e):
                replay(e, "pe")

            @block.scalar
            def _(e):
                replay(e, "act")

            @block.vector
            def _(e):
                replay(e, "dve")

            @block.gpsimd
            def _(e):
                replay(e, "pool")

            @block.sync
            def _(e):
                replay(e, "sp")
import ml_dtypes

BF = ml_dtypes.bfloat16
T = 4096
D = 2048
DEPTH = 2
NZ = 13872
ALPHA = (2 * DEPTH) ** 0.25
LN_EPS = 1e-5
ROW = dict(a_in=0, a_b=1024, a_c=2048, r_gate=3072, r_in=4096, q=5120, kc=6144, vc=6400,
           ks=6656, vs=6912, kw=7168, vw=7424, mg=7680, ng=13824)
NEGB = -30000.0

_sizes = [("IN", 108 * 128 * 16 * 128), ("INL", 128 * 16 * 48), ("BR", 16 * 128 * 3 * 8 * 128),
          ("OUT", 4 * 128 * 16 * 512), ("PG", 4 * 128 * 16 * 512), ("PLE", 4 * 128 * 2 * 512),
          ("GU", 16 * 8 * 128 * 2 * 16 * 128), ("DN", 16 * 4 * 128 * 8 * 512),
          ("LRU", 128 * 2 * 8 * 128), ("W1", 64 * 2 * 32 * 128), ("W2", 128 * 2 * 64), ("POS", 64 * 2 * 32)]
OFF = {}
_o = 0
for _n, _s in _sizes:
    OFF[_n] = _o
    _o += _s
NW = _o
assert NW % 128 == 0

_sp = [("CW", 24), ("CB", 8), ("LW", 32), ("LB", 8), ("BA", 8), ("BX", 8), ("LAM", 8), ("B1", 2),
       ("WR", 256), ("BR", 16), ("LNG", 4096), ("LNB", 4096)]
SP = {}
_o = 0
for _n, _s in _sp:
    SP[_n] = (_o, _s)
    _o += _s
NS = _o

CB_ID, CB_CAUS, CB_TAIL, CB_E, CB_CM, CB_MASK = 0, 128, 256, 384, 384 + 4096, 384 + 4096 + 128
CB_L = CB_MASK + 32 * 2 * 128
CB_ONES = CB_L + 128
NCB = CB_ONES + 128
CAP = 1024
I32 = mybir.dt.int32
CF_ID, CF_WC, CF_WF, CF_RB = 0, 128, 256, 384
NCF = 400


SEGS = [(0, OFF["BR"]), (OFF["BR"], OFF["GU"]), (OFF["GU"], OFF["DN"]), (OFF["DN"], NW)]


def wview(wb, name, idx, pat, **kw):
    bi, bs = idx
    off = OFF[name] + bi * bs
    for si, (s0, s1) in enumerate(SEGS):
        if s0 <= off < s1:
            return wb[si][off - s0:off - s0 + bs].rearrange(pat, **kw)
    raise ValueError(name)


def cast_jobs(src_all, wb, seg_ids, W=8192):
    jobs = []
    for si in seg_ids:
        s0, s1 = SEGS[si]
        F = (s1 - s0) // 128
        src = src_all[s0:s1].rearrange("(p f) -> p f", p=128)
        dst = wb[si].rearrange("(p f) -> p f", p=128)
        for f0 in range(0, F, W):
            jobs.append((src, dst, f0, min(W, F - f0)))
    return jobs


def st_cast(kb, jobs):
    W = 8192
    with ExitStack() as st:
        fb = [kb.sb(st, "cf%d" % i, [128, W], F32) for i in range(3)]
        bb = [kb.sb(st, "cb%d" % i, [128, W], BF16) for i in range(3)]
        for i, (src, dst, f0, w) in enumerate(jobs):
            s = i % 3
            kb.dma("sp", fb[s][:, :w], src[:, f0:f0 + w], writes=[fb[s]])
            if i % 2 == 0:
                kb.op("dve", lambda e, s=s, w=w: e.tensor_copy(out=bb[s][:, :w], in_=fb[s][:, :w]),
                      reads=[fb[s]], writes=[bb[s]])
            else:
                kb.op("act", lambda e, s=s, w=w: e.activation(out=bb[s][:, :w], in_=fb[s][:, :w], func=AF.Copy),
                      reads=[fb[s]], writes=[bb[s]])
            kb.dma("pool", dst[:, f0:f0 + w], bb[s][:, :w], reads=[bb[s]])
        kb.flush()


def st_inproj(kb, xin, wb, zT, cf32, bg_jobs=()):
    with ExitStack() as st:
        BW = 8192
        bg = {"next_load": 0, "next_cast": 0}
        if bg_jobs:
            fbg = [kb.sb(st, "fbg%d" % i, [128, BW], F32) for i in range(2)]
            bbg = [kb.sb(st, "bbg%d" % i, [128, BW], BF16) for i in range(2)]

        def bg_load():
            i = bg["next_load"]
            if i >= len(bg_jobs):
                return
            src, dst, f0, w = bg_jobs[i]
            kb.dma("pool", fbg[i % 2][:, :w], src[:, f0:f0 + w], writes=[fbg[i % 2]])
            bg["next_load"] += 1

        def bg_step():
            i = bg["next_cast"]
            if i >= len(bg_jobs):
                return
            bg_load()
            src, dst, f0, w = bg_jobs[i]
            s_ = i % 2
            eng = ("dve", "act")[i % 2]
            if eng == "act":
                kb.op("act", lambda e, s_=s_, w=w: e.activation(out=bbg[s_][:, :w], in_=fbg[s_][:, :w], func=AF.Copy),
                      reads=[fbg[s_]], writes=[bbg[s_]])
            else:
                kb.op(eng, lambda e, s_=s_, w=w: e.tensor_copy(out=bbg[s_][:, :w], in_=fbg[s_][:, :w]),
                      reads=[fbg[s_]], writes=[bbg[s_]])
            kb.dma("pool", dst[:, f0:f0 + w], bbg[s_][:, :w], reads=[bbg[s_]])
            bg["next_cast"] += 1
        if bg_jobs:
            bg_load()
        ident = kb.sb(st, "ident", [128, 128], F32)
        kb.dma("sp", ident[:], cf32[:, CF_ID:CF_ID + 128], writes=[ident])
        xT = kb.sb(st, "xT", [128, 16, 2048], BF16)
        xt = [kb.sb(st, "xt%d" % i, [128, 2048], F32) for i in range(2)]
        ptr = [kb.ps(st, "ptr%d" % i, [128, 4, 128], F32) for i in range(2)]
        pacc = [kb.ps(st, "pacc%d" % i, [128, 512], F32) for i in range(4)]
        wp = [kb.sb(st, "wp%d" % i, [128, 16, 128], BF16) for i in range(3)]
        ob = [kb.sb(st, "ob%d" % i, [128, 2048], BF16) for i in range(2)]
        ev = 0
        for half in range(2):
            for tt in range(16):
                tok0 = half * 2048 + tt * 128
                x_ = xt[tt % 2]
                kb.dma("sp", x_[:], xin[tok0:tok0 + 128, :], writes=[x_])
                for k4 in range(4):
                    pb = ptr[(tt * 4 + k4) % 2]
                    for j in range(4):
                        kc = k4 * 4 + j
                        kb.op("pe", lambda e, pb=pb, j=j, x_=x_, kc=kc: e.transpose(
                            out=pb[:, j, :], in_=x_[:, kc * 128:(kc + 1) * 128], identity=ident[:]),
                            reads=[x_, ident], writes=[pb], wadd=(j > 0))
                    oap = xT[:, k4 * 4:(k4 + 1) * 4, tt * 128:(tt + 1) * 128]
                    first = (tt == 0 and k4 == 0)
                    if ev % 2 == 0:
                        kb.op("act", lambda e, oap=oap, pb=pb: e.activation(out=oap, in_=pb[:], func=AF.Copy),
                              reads=[pb], writes=[xT], wadd=not first)
                    else:
                        kb.op("dve", lambda e, oap=oap, pb=pb: e.tensor_copy(out=oap, in_=pb[:]),
                              reads=[pb], writes=[xT], wadd=not first)
                    ev += 1
            for pn in range(109):
                if bg_jobs and pn % 2 == 0:
                    bg_step()
                M = 128 if pn < 108 else 48
                w = wp[pn % 3]
                if pn < 108:
                    src = wview(wb, "IN", (pn, 128 * 16 * 128), "(p k n) -> p k n", p=128, k=16)
                else:
                    src = wview(wb, "INL", (0, 128 * 16 * 48), "(p k n) -> p k n", p=128, k=16)
                kb.dma("sp", w[:, :, :M], src, writes=[w])
                o = ob[pn % 2]
                r0 = pn * 128
                if r0 >= ROW["mg"]:
                    func, scale = AF.Sigmoid, 1.0
                elif ROW["q"] <= r0 < ROW["kc"]:
                    func, scale = AF.Copy, 0.125
                else:
                    func, scale = AF.Copy, 1.0
                for tb in range(4):
                    pa = pacc[(pn * 4 + tb) % 4]
                    for kc in range(16):
                        kb.op("pe", lambda e, pa=pa, w=w, kc=kc, tb=tb, M=M: e.matmul(
                            pa[:M, :], lhsT=w[:, kc, :M], rhs=xT[:, kc, tb * 512:(tb + 1) * 512],
                            start=(kc == 0), stop=(kc == 15)),
                            reads=[w, xT], writes=[pa], wadd=(kc > 0))
                    oap = o[:M, tb * 512:(tb + 1) * 512]
                    if func == AF.Sigmoid or scale != 1.0 or ev % 2 == 0:
                        kb.op("act", lambda e, oap=oap, pa=pa, M=M, func=func, scale=scale: e.activation(
                            out=oap, in_=pa[:M, :], func=func, scale=scale),
                            reads=[pa], writes=[o], wadd=(tb > 0))
                    else:
                        kb.op("dve", lambda e, oap=oap, pa=pa, M=M: e.tensor_copy(out=oap, in_=pa[:M, :]),
                              reads=[pa], writes=[o], wadd=(tb > 0))
                    ev += 1
                kb.dma("pool", zT[r0:r0 + M, half * 2048:(half + 1) * 2048], o[:M, :], reads=[o])
        while bg_jobs and bg["next_cast"] < len(bg_jobs):
            bg_step()
        kb.flush()


def st_conv(kb, zT, yT, spl):
    with ExitStack() as st:
        cw = kb.sb(st, "cw", [128, 32], F32)
        kb.dma("sp", cw[:], spl[:, SP["CW"][0]:SP["CW"][0] + 32], writes=[cw])
        ain = [kb.sb(st, "ain%d" % i, [128, T], BF16) for i in range(2)]
        ab = [kb.sb(st, "ab%d" % i, [128, T], BF16) for i in range(2)]
        ac = [kb.sb(st, "ac%d" % i, [128, T], BF16) for i in range(2)]
        u = [kb.sb(st, "u%d" % i, [128, T + 2], F32) for i in range(2)]
        acc = [kb.sb(st, "acc%d" % i, [128, T], F32) for i in range(2)]
        yo = [kb.sb(st, "yo%d" % i, [128, T], BF16) for i in range(2)]
        for i in range(2):
            kb.op("dve", lambda e, i=i: e.memset(u[i][:, 0:2], 0.0), writes=[u[i]])
        for c in range(8):
            s = c % 2
            eng = "dve"
            kb.dma("sp", ain[s][:], zT[ROW["a_in"] + c * 128:ROW["a_in"] + (c + 1) * 128, :], writes=[ain[s]])
            kb.dma("sp", ab[s][:], zT[ROW["a_b"] + c * 128:ROW["a_b"] + (c + 1) * 128, :], writes=[ab[s]])
            kb.dma("sp", ac[s][:], zT[ROW["a_c"] + c * 128:ROW["a_c"] + (c + 1) * 128, :], writes=[ac[s]])
            us, accs, ys = u[s], acc[s], yo[s]
            kb.op("pool", lambda e, us=us, s=s: e.tensor_tensor(out=us[:, 2:T + 2], in0=ac[s][:], in1=ain[s][:], op=ALU.mult),
                  reads=[ac[s], ain[s]], writes=[us], wadd=True)
            kb.op(eng, lambda e, us=us, accs=accs, c=c: e.tensor_scalar(
                out=accs[:], in0=us[:, 0:T], scalar1=cw[:, c * 3:c * 3 + 1], scalar2=cw[:, 24 + c:25 + c],
                op0=ALU.mult, op1=ALU.add), reads=[us, cw], writes=[accs])
            for j in (1, 2):
                kb.op(eng, lambda e, us=us, accs=accs, c=c, j=j: e.scalar_tensor_tensor(
                    out=accs[:], in0=us[:, j:T + j], scalar=cw[:, c * 3 + j:c * 3 + j + 1], in1=accs[:],
                    op0=ALU.mult, op1=ALU.add), reads=[us, cw, accs], writes=[accs])
            kb.op("pool", lambda e, ys=ys, accs=accs, s=s: e.tensor_tensor(out=ys[:], in0=accs[:], in1=ab[s][:], op=ALU.mult),
                  reads=[accs, ab[s]], writes=[ys])
            kb.dma("pool" if s == 0 else "sp", yT[c * 128:(c + 1) * 128, :], ys[:], reads=[ys])
        kb.flush()


def st_lru(kb, zT, yT, spl, wb):
    with ExitStack() as st:
        o0 = SP["LW"][0]
        n0 = SP["LAM"][0] + 8 - o0
        sm = kb.sb(st, "sm", [128, n0], F32)
        kb.dma("sp", sm[:], spl[:, o0:o0 + n0], writes=[sm])
        LW, LB, BA, BX, LAM = 0, 32, 40, 48, 56
        lw = kb.sb(st, "lruw", [128, 2, 8, 128], BF16)
        kb.dma("sp", lw[:], wview(wb, "LRU", (0, 128 * 2048), "(p a h j) -> p a h j", p=128, a=2, h=8), writes=[lw])
        cv = kb.sb(st, "cv", [128, 3, 8], F32)
        kb.op("act", lambda e: e.activation(out=cv[:, 0, :], in_=sm[:, LAM:LAM + 8], func=AF.Exp, scale=-1.0),
              reads=[sm], writes=[cv])
        kb.op("act", lambda e: e.activation(out=cv[:, 0, :], in_=cv[:, 0, :], func=AF.Ln, bias=1.0),
              reads=[cv], writes=[cv])
        kb.op("dve", lambda e: e.tensor_scalar(out=cv[:, 1, :], in0=cv[:, 0, :], scalar1=-8.0, scalar2=None, op0=ALU.mult),
              reads=[cv], writes=[cv])
        kb.op("dve", lambda e: e.tensor_scalar(out=cv[:, 2, :], in0=cv[:, 0, :], scalar1=-16.0, scalar2=None, op0=ALU.mult),
              reads=[cv], writes=[cv])
        rin = kb.sb(st, "rin", [128, T], BF16)
        rg = kb.sb(st, "rg", [128, T], BF16)
        up = kb.sb(st, "up", [128, T + 3], F32)
        xc = kb.sb(st, "xc", [128, T], F32)
        xcb = kb.sb(st, "xcb", [128, T], BF16)
        r = kb.sb(st, "r", [128, T], F32)
        ii = kb.sb(st, "ii", [128, T], F32)
        a = kb.sb(st, "a", [128, T], F32)
        a2 = kb.sb(st, "a2", [128, T], F32)
        yo = kb.sb(st, "yo", [128, T], BF16)
        pr = [kb.ps(st, "pr%d" % i, [128, 512], F32) for i in range(4)]
        kb.op("dve", lambda e: e.memset(up[:, 0:3], 0.0), writes=[up])
        for h in range(8):
            kb.dma("sp", rin[:], zT[ROW["r_in"] + h * 128:ROW["r_in"] + (h + 1) * 128, :], writes=[rin])
            kb.dma("sp", rg[:], zT[ROW["r_gate"] + h * 128:ROW["r_gate"] + (h + 1) * 128, :], writes=[rg])
            kb.op("pool", lambda e: e.tensor_copy(out=up[:, 3:T + 3], in_=rin[:]), reads=[rin], writes=[up], wadd=True)
            kb.op("dve", lambda e, h=h: e.tensor_scalar(
                out=xc[:], in0=up[:, 0:T], scalar1=sm[:, LW + h * 4:LW + h * 4 + 1], scalar2=sm[:, LB + h:LB + h + 1],
                op0=ALU.mult, op1=ALU.add), reads=[up, sm], writes=[xc])
            for j in (1, 2, 3):
                kb.op("dve", lambda e, h=h, j=j: e.scalar_tensor_tensor(
                    out=xc[:], in0=up[:, j:T + j], scalar=sm[:, LW + h * 4 + j:LW + h * 4 + j + 1], in1=xc[:],
                    op0=ALU.mult, op1=ALU.add), reads=[up, sm, xc], writes=[xc])
            kb.op("act", lambda e: e.activation(out=xcb[:], in_=xc[:], func=AF.Copy), reads=[xc], writes=[xcb])
            for tb in range(8):
                pa, px = pr[(tb * 2) % 4], pr[(tb * 2 + 1) % 4]
                sl = slice(tb * 512, (tb + 1) * 512)
                kb.op("pe", lambda e, pa=pa, h=h, sl=sl: e.matmul(pa[:], lhsT=lw[:, 0, h, :], rhs=xcb[:, sl], start=True, stop=True),
                      reads=[lw, xcb], writes=[pa])
                kb.op("pe", lambda e, px=px, h=h, sl=sl: e.matmul(px[:], lhsT=lw[:, 1, h, :], rhs=xcb[:, sl], start=True, stop=True),
                      reads=[lw, xcb], writes=[px])
                kb.op("act", lambda e, pa=pa, h=h, sl=sl: e.activation(out=r[:, sl], in_=pa[:], func=AF.Sigmoid, bias=sm[:, BA + h:BA + h + 1]),
                      reads=[pa, sm], writes=[r], wadd=(tb > 0))
                kb.op("act", lambda e, px=px, h=h, sl=sl: e.activation(out=ii[:, sl], in_=px[:], func=AF.Sigmoid, bias=sm[:, BX + h:BX + h + 1]),
                      reads=[px, sm], writes=[ii], wadd=(tb > 0))
            kb.op("act", lambda e, h=h: e.activation(out=a[:], in_=r[:], func=AF.Exp, scale=cv[:, 1, h:h + 1]),
                  reads=[r, cv], writes=[a])
            kb.op("act", lambda e, h=h: e.activation(out=a2[:], in_=r[:], func=AF.Exp, scale=cv[:, 2, h:h + 1]),
                  reads=[r, cv], writes=[a2])
            kb.op("dve", lambda e: e.tensor_scalar(out=a2[:], in0=a2[:], scalar1=1.0, scalar2=-1.0, op0=ALU.min, op1=ALU.mult),
                  reads=[a2], writes=[a2])
            kb.op("act", lambda e: e.activation(out=a2[:], in_=a2[:], func=AF.Sqrt, bias=1.0, scale=1.0), reads=[a2], writes=[a2])
            kb.op("pool", lambda e: e.tensor_tensor(out=ii[:], in0=ii[:], in1=xc[:], op=ALU.mult), reads=[ii, xc], writes=[ii])
            kb.op("dve", lambda e: e.tensor_tensor(out=a2[:], in0=a2[:], in1=ii[:], op=ALU.mult), reads=[a2, ii], writes=[a2])
            kb.op("dve", lambda e: e.tensor_tensor_scan(out=r[:], data0=a[:], data1=a2[:], initial=0.0, op0=ALU.mult, op1=ALU.add),
                  reads=[a, a2], writes=[r])
            kb.op("act", lambda e: e.activation(out=xc[:], in_=rg[:], func=AF.Gelu_apprx_tanh), reads=[rg], writes=[xc])
            kb.op("dve", lambda e: e.tensor_tensor(out=yo[:], in0=r[:], in1=xc[:], op=ALU.mult), reads=[r, xc], writes=[yo])
            kb.dma("pool", yT[1024 + h * 128:1024 + (h + 1) * 128, :], yo[:], reads=[yo])
        kb.flush()


def st_nsa(kb, zT, yT, wb, spl, cbf, cf32, kaug, kaugc, qaug):
    with ExitStack() as st:
        cb = kb.sb(st, "cbt", [128, NCB], BF16)
        kb.dma("sp", cb[:], cbf, writes=[cb])
        cf = kb.sb(st, "cft", [128, NCF], F32)
        kb.dma("sp", cf[:], cf32, writes=[cf])
        b1 = kb.sb(st, "b1", [128, 2], F32)
        kb.dma("sp", b1[:], spl[:, SP["B1"][0]:SP["B1"][0] + 2], writes=[b1])
        identb = cb[:, CB_ID:CB_ID + 128]
        caus4 = cb[:, CB_CAUS:CB_CAUS + 128].unsqueeze(1).broadcast_to([128, 4, 128])
        tail4 = cb[:, CB_TAIL:CB_TAIL + 128].unsqueeze(1).broadcast_to([128, 4, 128])
        ksT = kb.sb(st, "ksT", [69, 4, T], BF16)
        kwT = kb.sb(st, "kwT", [69, 4, T], BF16)
        kcT = kb.sb(st, "kcT", [69, 4, 256], BF16)
        Vs = kb.sb(st, "Vs", [128, 32, 4, 65], BF16)
        Vw = kb.sb(st, "Vw", [128, 32, 4, 65], BF16)
        Vc = kb.sb(st, "Vc", [128, 2, 4, 65], BF16)
        gT = kb.sb(st, "gT", [48, T], BF16)
        zr = kb.sb(st, "zr", [1, 512], BF16)
        st2 = ExitStack()
        big = kb.sb(st2, "big", [128, 4, T], BF16)
        w1 = kb.sb(st2, "w1", [64, 2, 32, 128], BF16)
        w2 = kb.sb(st2, "w2", [128, 2, 64], BF16)
        posT = kb.sb(st2, "posT", [64, 2, 32], BF16)
        hid = kb.sb(st2, "hid", [128, 256], BF16)
        bt = kb.sb(st2, "bt", [128, 2], F32)
        S = [kb.ps(st, "S%d" % i, [128, 4, 128], F32) for i in range(2)]
        OA = [kb.ps(st, "OA%d" % i, [128, 4, 128], F32) for i in range(2)]
        IB = kb.ps(st, "IB", [128, 4, 128], F32)
        MB = kb.ps(st, "MB", [128, 8, 128], BF16)
        MF = kb.ps(st, "MF", [128, 512], F32)

        kb.op("dve", lambda e: e.memset(zr[:], 0.0), writes=[zr])
        kb.op("dve", lambda e: e.memset(hid[:], 0.0), writes=[hid])
        for V in (Vs, Vw, Vc):
            kb.op("pool", lambda e, V=V: e.memset(V[:, :, :, 64:65], 1.0), writes=[V])
        kb.dma("sp", ksT[0:64, :, :], zT[ROW["ks"]:ROW["ks"] + 256, :].rearrange("(g d) t -> d g t", d=64), writes=[ksT])
        kb.dma("sp", ksT[64:69, :, :], kaug, writes=[ksT], wadd=True)
        kb.dma("sp", kwT[0:64, :, :], zT[ROW["kw"]:ROW["kw"] + 256, :].rearrange("(g d) t -> d g t", d=64), writes=[kwT])
        kb.dma("sp", kwT[64:69, :, :], kaug, writes=[kwT], wadd=True)
        kb.dma("sp", kcT[64:69, :, :], kaugc, writes=[kcT])
        kb.dma("sp", gT[:], zT[ROW["ng"]:ROW["ng"] + 48, :], writes=[gT])
        kb.dma("sp", w1[:], wview(wb, "W1", (0, 64 * 8192), "(p s j h) -> p s j h", p=64, s=2, j=32), writes=[w1])
        kb.dma("sp", w2[:], wview(wb, "W2", (0, 128 * 128), "(p s d) -> p s d", p=128, s=2), writes=[w2])
        kb.dma("sp", posT[:], wview(wb, "POS", (0, 64 * 64), "(p s j) -> p s j", p=64, s=2), writes=[posT])
        for (V, rname) in ((Vs, "vs"), (Vw, "vw")):
            kb.dma("sp", big[:, 0:2, :], zT[ROW[rname]:ROW[rname] + 256, :].rearrange("(a p) t -> p a t", p=128), writes=[big])
            for k4 in range(8):
                for kk in range(4):
                    kt = k4 * 4 + kk
                    for a_ in range(2):
                        kb.op("pe", lambda e, kk=kk, a_=a_, kt=kt: e.transpose(
                            out=MB[:, kk * 2 + a_, :], in_=big[:, a_, kt * 128:(kt + 1) * 128], identity=identb),
                            reads=[big, cb], writes=[MB], wadd=not (kk == 0 and a_ == 0))
                kb.op("dve" if k4 % 2 == 0 else "act",
                      (lambda e, V=V, k4=k4: e.tensor_copy(
                          out=V[:, k4 * 4:(k4 + 1) * 4, :, 0:64],
                          in_=MB[:].rearrange("p (k a) (g d) -> p k (a g) d", k=4, g=2))) if k4 % 2 == 0 else
                      (lambda e, V=V, k4=k4: e.activation(
                          out=V[:, k4 * 4:(k4 + 1) * 4, :, 0:64],
                          in_=MB[:].rearrange("p (k a) (g d) -> p k (a g) d", k=4, g=2), func=AF.Copy)),
                      reads=[MB], writes=[V], wadd=True)
        for s in range(2):
            for j in range(32):
                kb.op("pe", lambda e, s=s, j=j: e.matmul(MF[:, s:s + 1], lhsT=w1[:, s, j, :], rhs=posT[:, s, j:j + 1],
                                                         start=(j == 0), stop=(j == 31)),
                      reads=[w1, posT], writes=[MF], wadd=not (s == 0 and j == 0))
        kb.op("dve", lambda e: e.tensor_tensor(out=bt[:], in0=MF[:, 0:2], in1=b1[:], op=ALU.add), reads=[MF, b1], writes=[bt])
        for s, rname in ((0, "kc"), (1, "vc")):
            kb.dma("sp", big[0:64, :, :], zT[ROW[rname]:ROW[rname] + 256, :].rearrange("(g d) t -> d g t", d=64), writes=[big])
            for g in range(4):
                for j in range(32):
                    kb.op("pe", lambda e, s=s, j=j, g=g: e.matmul(
                        MF[:, 0:255], lhsT=w1[:, s, j, :], rhs=big[0:64, g, j:j + 16 * 254 + 1:16],
                        start=(j == 0), stop=(j == 31)), reads=[w1, big], writes=[MF], wadd=(j > 0))
                kb.op("act", lambda e, s=s: e.activation(out=hid[:, 0:255], in_=MF[:, 0:255], func=AF.Gelu_apprx_tanh,
                                                         bias=bt[:, s:s + 1]), reads=[MF, bt], writes=[hid])
                if s == 0:
                    kb.op("pe", lambda e: e.matmul(MF[0:64, 256:512], lhsT=w2[:, 0, :], rhs=hid[:], start=True, stop=True),
                          reads=[w2, hid], writes=[MF])
                    kb.op("dve", lambda e, g=g: e.tensor_copy(out=kcT[0:64, g, :], in_=MF[0:64, 256:512]),
                          reads=[MF], writes=[kcT], wadd=True)
                else:
                    for nt in range(2):
                        kb.op("pe", lambda e, nt=nt: e.matmul(MF[:, 256 + nt * 64:256 + (nt + 1) * 64],
                                                              lhsT=hid[:, nt * 128:(nt + 1) * 128], rhs=w2[:, 1, :],
                                                              start=True, stop=True), reads=[w2, hid], writes=[MF], wadd=(nt > 0))
                    kb.op("dve", lambda e, g=g: e.tensor_copy(
                        out=Vc[:, :, g, 0:64], in_=MF[:, 256:384].rearrange("p (n d) -> p n d", n=2)),
                        reads=[MF], writes=[Vc], wadd=True)

        kb.flush()
        st2.close()
        qT = [kb.sb(st, "qT%d" % i, [69, 16, 128], BF16) for i in range(2)]
        gt = [kb.sb(st, "gt%d" % i, [128, 16, 3], F32) for i in range(2)]
        yc = [kb.sb(st, "yc%d" % i, [128, 1024], F32) for i in range(2)]
        ycb = kb.sb(st, "ycb", [128, 1024], BF16)
        ycT = [kb.sb(st, "ycT%d" % i, [128, 8, 128], BF16) for i in range(2)]
        pT = [kb.sb(st, "pT%d" % i, [128, 4, 128], BF16) for i in range(3)]
        mbT = [kb.sb(st, "mbT%d" % i, [64, 128], BF16) for i in range(2)]
        rd = kb.sb(st, "rd", [128, 4], F32)
        coef = kb.sb(st, "coef", [128, 4], F32)
        imp = kb.sb(st, "imp", [128, 64], F32)
        impm = kb.sb(st, "impm", [128, 64], F32)
        wk = kb.sb(st, "wk", [128, 64], F32)
        m8 = kb.sb(st, "m8", [128, 8], F32)
        m8b = kb.sb(st, "m8b", [128, 8], F32)
        mb = kb.sb(st, "mb", [128, 64], BF16)
        cnt = {"S": 0, "OA": 0, "p": 0, "mb": 0}

        pend = []

        def drain():
            for f in pend:
                f()
            pend.clear()

        def zero_init(P, ncol):
            kb.op("pe", lambda e, P=P, ncol=ncol: e.matmul(
                P[:, :, 0:ncol], lhsT=zr[0:1, 0:128], rhs=zr[0:1, 0:4 * ncol].rearrange("p (a b) -> p a b", a=4),
                start=True, stop=False, skip_group_check=True), reads=[zr], writes=[P])

        def unit(q, g, lhs_main, extra, V_ap, P, IBp=None, CM_ap=None, last=False):
            Sb = S[cnt["S"] % 2]
            cnt["S"] += 1
            n_ex = len(extra)
            kb.op("pe", lambda e, Sb=Sb, lhs_main=lhs_main, q=q, g=g, n_ex=n_ex: e.matmul(
                Sb[:], lhsT=lhs_main, rhs=q[:, 4 * g:4 * g + 4, :], start=True, stop=(n_ex == 0)),
                reads=[q, ksT, kwT, kcT], writes=[Sb])
            for i, (l_, r_, rb) in enumerate(extra):
                kb.op("pe", lambda e, Sb=Sb, l_=l_, r_=r_, i=i, n_ex=n_ex: e.matmul(
                    Sb[:], lhsT=l_, rhs=r_, start=False, stop=(i == n_ex - 1)),
                    reads=[cb] + rb, writes=[Sb], wadd=True)
            p_ = pT[cnt["p"] % 3]
            cnt["p"] += 1
            kb.op("act", lambda e, p_=p_, Sb=Sb: e.activation(out=p_[:], in_=Sb[:], func=AF.Exp), reads=[Sb], writes=[p_])
            drain()

            def pv(p_=p_, P=P, V_ap=V_ap, IBp=IBp, CM_ap=CM_ap, last=last):
                for h in range(4):
                    kb.op("pe", lambda e, P=P, h=h, p_=p_, V_ap=V_ap: e.matmul(
                        P[:, h, 0:65], lhsT=p_[:, h, :], rhs=V_ap, start=False, stop=(last and h == 3 and IBp is None),
                        skip_group_check=True), reads=[p_, Vs, Vw, Vc], writes=[P], wadd=True)
                    if IBp is not None:
                        kb.op("pe", lambda e, IBp=IBp, h=h, p_=p_, CM_ap=CM_ap: e.matmul(
                            IBp[:, h, 0:64], lhsT=p_[:, h, :], rhs=CM_ap, start=False, stop=(last and h == 3),
                            skip_group_check=True), reads=[p_, cb], writes=[IBp], wadd=True)
            pend.append(pv)

        def finalize(P, g, br, gt_, yc_):
            kb.op("dve", lambda e, P=P: e.tensor_scalar(out=rd[:], in0=P[:, :, 64], scalar1=1e-30, scalar2=None, op0=ALU.max),
                  reads=[P], writes=[rd])
            kb.op("dve", lambda e: e.reciprocal(out=rd[:], in_=rd[:]), reads=[rd], writes=[rd])
            kb.op("dve", lambda e, g=g, br=br, gt_=gt_: e.tensor_tensor(out=coef[:], in0=rd[:], in1=gt_[:, 4 * g:4 * g + 4, br], op=ALU.mult),
                  reads=[rd, gt_], writes=[coef])
            for h in range(4):
                c0 = (4 * g + h) * 64
                if br == 0:
                    kb.op("dve", lambda e, P=P, h=h, c0=c0, yc_=yc_: e.tensor_scalar(
                        out=yc_[:, c0:c0 + 64], in0=P[:, h, 0:64], scalar1=coef[:, h:h + 1], scalar2=None, op0=ALU.mult),
                        reads=[P, coef], writes=[yc_], wadd=True)
                else:
                    kb.op("dve", lambda e, P=P, h=h, c0=c0, yc_=yc_: e.scalar_tensor_tensor(
                        out=yc_[:, c0:c0 + 64], in0=P[:, h, 0:64], scalar=coef[:, h:h + 1], in1=yc_[:, c0:c0 + 64],
                        op0=ALU.mult, op1=ALU.add), reads=[P, coef, yc_], writes=[yc_], wadd=True)

        for a in range(32):
            q = qT[a % 2]
            gt_ = gt[a % 2]
            yc_ = yc[a % 2]
            tsl = slice(a * 128, (a + 1) * 128)
            kb.dma("sp", q[0:64, :, :], zT[ROW["q"]:ROW["q"] + 1024, tsl].rearrange("(h d) t -> d h t", d=64), writes=[q])
            kb.dma("sp", q[64:69, :, :], qaug[:, :, tsl], writes=[q], wadd=True)
            kb.op("pe", lambda e, tsl=tsl: e.transpose(out=MB[:, 0, 0:48], in_=gT[:, tsl], identity=identb[0:48, 0:48]),
                  reads=[gT, cb], writes=[MB])
            kb.op("dve", lambda e, gt_=gt_: e.tensor_copy(out=gt_[:].rearrange("p h b -> p (h b)"), in_=MB[:, 0, 0:48]),
                  reads=[MB], writes=[gt_])
            for g in range(4):
                P = OA[cnt["OA"] % 2]
                cnt["OA"] += 1
                zero_init(P, 65)
                zero_init(IB, 64)
                ntc = 1 if a < 16 else 2
                for nt in range(ntc):
                    cm4 = cb[:, CB_MASK + (a * 2 + nt) * 128:CB_MASK + (a * 2 + nt + 1) * 128].unsqueeze(1).broadcast_to([128, 4, 128])
                    unit(q, g, kcT[:, g, nt * 128:(nt + 1) * 128], [(identb, cm4, [])], Vc[:, nt, g, :], P,
                         IBp=IB, CM_ap=cb[:, CB_CM + nt * 64:CB_CM + (nt + 1) * 64], last=(nt == ntc - 1))
                drain()
                finalize(P, g, 0, gt_, yc_)
                for h in range(4):
                    if h == 0:
                        kb.op("dve", lambda e: e.tensor_scalar(out=imp[:], in0=IB[:, 0, 0:64], scalar1=rd[:, 0:1], scalar2=None, op0=ALU.mult),
                              reads=[IB, rd], writes=[imp])
                    else:
                        kb.op("dve", lambda e, h=h: e.scalar_tensor_tensor(out=imp[:], in0=IB[:, h, 0:64], scalar=rd[:, h:h + 1], in1=imp[:],
                                                                         op0=ALU.mult, op1=ALU.add), reads=[IB, rd, imp], writes=[imp])
                j0 = 63 - 2 * a
                kb.op("dve", lambda e, j0=j0: e.tensor_tensor(out=impm[:], in0=imp[:], in1=cf[:, CF_WC + j0:CF_WC + j0 + 64], op=ALU.mult),
                      reads=[imp, cf], writes=[impm])
                kb.op("dve", lambda e, j0=j0: e.tensor_tensor(out=impm[:], in0=impm[:], in1=cf[:, CF_WF + j0:CF_WF + j0 + 64], op=ALU.add),
                      reads=[impm, cf], writes=[impm])
                kb.op("dve", lambda e: e.memset(impm[:, 0:1], 1e9), reads=[impm], writes=[impm])
                kb.op("dve", lambda e: e.max(out=m8[:], in_=impm[:]), reads=[impm], writes=[m8])
                kb.op("dve", lambda e: e.match_replace(out=wk[:], in_to_replace=m8[:], in_values=impm[:], imm_value=-3e9),
                      reads=[impm, m8], writes=[wk])
                kb.op("dve", lambda e: e.max(out=m8b[:], in_=wk[:]), reads=[wk], writes=[m8b])
                kb.op("dve", lambda e: e.tensor_scalar(out=wk[:], in0=impm[:], scalar1=m8b[:, 7:8], scalar2=None, op0=ALU.is_ge),
                      reads=[impm, m8b, wk], writes=[wk])
                kb.op("dve", lambda e: e.tensor_scalar(out=mb[:], in0=wk[:], scalar1=-NEGB, scalar2=NEGB, op0=ALU.mult, op1=ALU.add),
                      reads=[wk], writes=[mb])
                mbT_ = mbT[cnt["mb"] % 2]
                cnt["mb"] += 1
                kb.op("pe", lambda e: e.transpose(out=MB[0:64, 1, :], in_=mb[:], identity=identb), reads=[mb, cb], writes=[MB])
                kb.op("dve", lambda e, mbT_=mbT_: e.tensor_copy(out=mbT_[:], in_=MB[0:64, 1, :]), reads=[MB], writes=[mbT_])
                P = OA[cnt["OA"] % 2]
                cnt["OA"] += 1
                zero_init(P, 65)
                b0 = max(0, a - 4)
                for b in range(b0, a + 1):
                    ex = []
                    if b == a:
                        ex.append((identb, caus4, []))
                    if b == a - 4:
                        ex.append((identb, tail4, []))
                    unit(q, g, kwT[:, g, b * 128:(b + 1) * 128], ex, Vw[:, b, g, :], P, last=(b == a))
                pend.append(lambda P=P, g=g, gt_=gt_, yc_=yc_: finalize(P, g, 2, gt_, yc_))
                P = OA[cnt["OA"] % 2]
                cnt["OA"] += 1
                zero_init(P, 65)
                mb4 = mbT_[:].unsqueeze(1).broadcast_to([64, 4, 128])
                for b in range(0, a + 1):
                    ex = [(cb[0:64, CB_E + b * 128:CB_E + (b + 1) * 128], mb4, [mbT_])]
                    if b == a:
                        ex.append((identb, caus4, []))
                    unit(q, g, ksT[:, g, b * 128:(b + 1) * 128], ex, Vs[:, b, g, :], P, last=(b == a))
                pend.append(lambda P=P, g=g, gt_=gt_, yc_=yc_: finalize(P, g, 1, gt_, yc_))
            drain()
            kb.op("act", lambda e, yc_=yc_: e.activation(out=ycb[:], in_=yc_[:], func=AF.Copy), reads=[yc_], writes=[ycb])
            for c in range(8):
                kb.op("pe", lambda e, c=c: e.transpose(out=MB[:, c, :], in_=ycb[:, c * 128:(c + 1) * 128], identity=identb),
                      reads=[ycb, cb], writes=[MB], wadd=(c > 0))
            ycT_ = ycT[a % 2]
            kb.op("dve", lambda e, ycT_=ycT_: e.tensor_copy(out=ycT_[:], in_=MB[:]), reads=[MB], writes=[ycT_])
            kb.dma("pool", yT[2048:3072, tsl].rearrange("(c p) t -> p c t", p=128), ycT_[:], reads=[ycT_])
        kb.flush()


def ln_tile(kb, r, rsl, lnp, outb, osl, tmp):
    bs, mv, rs = tmp
    lng, lnb = lnp[:, 0, :], lnp[:, 1, :]
    for c in range(4):
        kb.op("dve", lambda e, c=c: e.bn_stats(out=bs[:, c, :], in_=r[rsl][:, c * 512:(c + 1) * 512]),
              reads=[r], writes=[bs], wadd=(c > 0))
    kb.op("dve", lambda e: e.bn_aggr(out=mv[:], in_=bs[:]), reads=[bs], writes=[mv])
    import os
    LNCUT = int(os.environ.get("LNCUT", "9"))
    if LNCUT < 2:
        kb.op("dve", lambda e: e.tensor_copy(out=outb[osl], in_=r[rsl]), reads=[r], writes=[outb])
        return
    kb.op("dve", lambda e: e.tensor_scalar(out=rs[:], in0=mv[:, 1:2], scalar1=LN_EPS, scalar2=None, op0=ALU.add),
          reads=[mv], writes=[rs])
    kb.op("act", lambda e: e.activation(out=rs[:], in_=rs[:], func=AF.Sqrt), reads=[rs], writes=[rs])
    kb.op("dve", lambda e: e.reciprocal(out=rs[:], in_=rs[:]), reads=[rs], writes=[rs])
    if LNCUT < 3:
        kb.op("dve", lambda e: e.tensor_copy(out=outb[osl], in_=r[rsl]), reads=[r], writes=[outb])
        return
    kb.op("dve", lambda e: e.tensor_scalar(out=r[rsl], in0=r[rsl], scalar1=mv[:, 0:1], scalar2=rs[:, 0:1],
                                           op0=ALU.subtract, op1=ALU.mult), reads=[r, mv, rs], writes=[r], wadd=True)
    if LNCUT < 4:
        kb.op("dve", lambda e: e.tensor_copy(out=outb[osl], in_=r[rsl]), reads=[r], writes=[outb])
        return
    kb.op("dve", lambda e: e.tensor_tensor(out=r[rsl], in0=r[rsl], in1=lng, op=ALU.mult), reads=[r, lnp], writes=[r], wadd=True)
    kb.op("dve", lambda e: e.tensor_tensor(out=outb[osl], in0=r[rsl], in1=lnb, op=ALU.add), reads=[r, lnp], writes=[outb])


def st_mix(kb, xin, zT, yT, wb, spl, cf32, xcur1, x1T, gates, cbf, tokid, oob, lst, ridx_d, gsel_d, x1b_d):
    with ExitStack() as st:
        dl = Buf("lst", None)
        kb.dma("sp", lst.rearrange("(p f) o -> p (f o)", p=128), oob.rearrange("(p f) o -> p (f o)", p=128), writes=[dl])
        lo = kb.sb(st, "lo", [128, 256], BF16)
        kb.dma("sp", lo[:], cbf[:, CB_L:CB_L + 256], writes=[lo])
        rowb = kb.sb(st, "rowb", [128, 16], F32)
        kb.dma("sp", rowb[:], cf32[:, CF_RB:CF_RB + 16], writes=[rowb])
        tki = kb.sb(st, "tki", [128, 32], I32)
        kb.dma("sp", tki[:], tokid, writes=[tki])
        chb = kb.sb(st, "chb", [128, 16], BF16)
        r12i = [kb.sb(st, "r12i%d" % i, [128, 2], I32) for i in range(2)]
        g12 = [kb.sb(st, "g12_%d" % i, [128, 2], F32) for i in range(2)]
        x1b = [kb.sb(st, "x1b%d" % i, [128, 2048], BF16) for i in range(2)]
        tk1 = [kb.sb(st, "tk1_%d" % i, [128, 1], I32) for i in range(2)]
        r1c = [kb.sb(st, "r1c%d" % i, [128, 1], I32) for i in range(4)]
        ident = kb.sb(st, "ident", [128, 128], F32)
        kb.dma("sp", ident[:], cf32[:, CF_ID:CF_ID + 128], writes=[ident])
        lnp = kb.sb(st, "lnp", [128, 2, 2048], F32)
        kb.dma("sp", lnp[:, 0, :], spl[:, SP["LNG"][0]:SP["LNG"][0] + 2048], writes=[lnp])
        kb.dma("sp", lnp[:, 1, :], spl[:, SP["LNB"][0]:SP["LNB"][0] + 2048], writes=[lnp], wadd=True)
        wr = kb.sb(st, "wr", [128, 16, 16], F32)
        kb.dma("sp", wr[:], spl[:, SP["WR"][0]:SP["WR"][0] + 256].rearrange("p (k e) -> p k e", k=16), writes=[wr])
        brt = kb.sb(st, "brt", [128, 16], F32)
        kb.dma("sp", brt[:], spl[:, SP["BR"][0]:SP["BR"][0] + 16], writes=[brt])
        yTb = kb.sb(st, "yTb", [128, 24, 512], BF16)
        xt = kb.sb(st, "xt", [128, 4, 2048], F32)
        mT = kb.sb(st, "mT", [128, 16, 512], BF16)
        gl = [kb.sb(st, "gl%d" % i, [128, 3, 512], BF16) for i in range(2)]
        wbr = [kb.sb(st, "wbr%d" % i, [128, 3, 8, 128], BF16) for i in range(2)]
        tm = [kb.sb(st, "tm%d" % i, [128, 512], F32) for i in range(3)]
        wo = [kb.sb(st, "wo%d" % i, [128, 16, 512], BF16) for i in range(2)]
        x1 = [kb.sb(st, "x1_%d" % i, [128, 2048], F32) for i in range(2)]
        xf = kb.sb(st, "xf", [128, 16, 128], F32)
        x1Tb = kb.sb(st, "x1Tb", [128, 16, 512], BF16)
        bs = kb.sb(st, "bs", [128, 4, 6], F32)
        mv = kb.sb(st, "mv", [128, 2], F32)
        rs = kb.sb(st, "rs", [128, 1], F32)
        R = {n: kb.sb(st, "rt_" + n, sh, F32) for n, sh in
             [("aff", [128, 16]), ("sel", [128, 16]), ("m1", [128, 4]), ("eq", [128, 16]), ("sel2", [128, 16]),
              ("m2", [128, 4]), ("gm", [128, 1]), ("t1", [128, 1]), ("t2", [128, 1]), ("is1", [128, 16]),
              ("den", [128, 1]), ("is2", [128, 16]), ("ch", [128, 16]), ("pos", [128, 16]), ("ovf", [128, 16]),
              ("tmp", [128, 16]), ("r12", [128, 2]), ("g12", [128, 2]), ("cntb", [128, 16])]}
        gate = [kb.sb(st, "gate%d" % i, [128, 16], F32) for i in range(2)]
        pacc = [kb.ps(st, "pbr%d" % i, [128, 512], F32) for i in range(3)]
        pout = [kb.ps(st, "pout%d" % i, [128, 512], F32) for i in range(2)]
        ptr = [kb.ps(st, "ptr%d" % i, [128, 4, 128], F32) for i in range(2)]
        prt = kb.ps(st, "prt", [128, 512], F32)
        mgv = zT[ROW["mg"]:ROW["mg"] + 6144, :].rearrange("(i n p) t -> p i n t", i=3, p=128)
        trc = 0
        for tbk in range(8):
            tsl = slice(tbk * 512, (tbk + 1) * 512)
            kb.dma("sp", yTb[:], yT[0:3072, tsl].rearrange("(k p) t -> p k t", p=128), writes=[yTb])
            kb.dma("sp", xt[:], xin[tsl, :].rearrange("(a p) d -> p a d", p=128), writes=[xt])
            for nch in range(16):
                g_ = gl[nch % 2]
                w_ = wbr[nch % 2]
                kb.dma("sp", g_[:], mgv[:, :, nch, tsl], writes=[g_])
                kb.dma("sp", w_[:], wview(wb, "BR", (nch, 128 * 3 * 8 * 128), "(p i k n) -> p i k n", p=128, i=3, k=8), writes=[w_])
                for i in range(3):
                    for kc in range(8):
                        kb.op("pe", lambda e, i=i, kc=kc, w_=w_: e.matmul(
                            pacc[i][:], lhsT=w_[:, i, kc, :], rhs=yTb[:, i * 8 + kc, :], start=(kc == 0), stop=(kc == 7)),
                            reads=[w_, yTb], writes=[pacc[i]], wadd=(kc > 0))
                for i in range(3):
                    kb.op("dve", lambda e, i=i, g_=g_: e.tensor_tensor(out=tm[i][:], in0=pacc[i][:], in1=g_[:, i, :], op=ALU.mult),
                          reads=[pacc[i], g_], writes=[tm[i]])
                kb.op("pool", lambda e: e.tensor_tensor(out=tm[0][:], in0=tm[0][:], in1=tm[1][:], op=ALU.add),
                      reads=[tm[0], tm[1]], writes=[tm[0]])
                kb.op("pool", lambda e, nch=nch: e.tensor_tensor(out=mT[:, nch, :], in0=tm[0][:], in1=tm[2][:], op=ALU.add),
                      reads=[tm[0], tm[2]], writes=[mT], wadd=(nch > 0))
            for n4 in range(4):
                w_ = wo[n4 % 2]
                kb.dma("sp", w_[:], wview(wb, "OUT", (n4, 128 * 16 * 512), "(p k n) -> p k n", p=128, k=16), writes=[w_])
                for tt in range(4):
                    po = pout[(n4 * 4 + tt) % 2]
                    for kc in range(16):
                        kb.op("pe", lambda e, po=po, kc=kc, tt=tt, w_=w_: e.matmul(
                            po[:], lhsT=mT[:, kc, tt * 128:(tt + 1) * 128], rhs=w_[:, kc, :], start=(kc == 0), stop=(kc == 15)),
                            reads=[mT, w_], writes=[po], wadd=(kc > 0))
                    kb.op("dve", lambda e, po=po, tt=tt, n4=n4: e.scalar_tensor_tensor(
                        out=xt[:, tt, n4 * 512:(n4 + 1) * 512], in0=xt[:, tt, n4 * 512:(n4 + 1) * 512], scalar=ALPHA, in1=po[:],
                        op0=ALU.mult, op1=ALU.add), reads=[xt, po], writes=[xt], wadd=True)

            def phase3(tt):
                nonlocal trc
                x1_ = x1[tt % 2]
                tok0 = tbk * 512 + tt * 128
                ln_tile(kb, xt, (slice(None), tt, slice(None)), lnp, x1_, (slice(None), slice(None)), (bs, mv, rs))
                kb.dma("pool", xcur1[tok0:tok0 + 128, :], x1_[:], reads=[x1_])
                for k4 in range(4):
                    pb = ptr[trc % 2]
                    trc += 1
                    for j in range(4):
                        kc = k4 * 4 + j
                        kb.op("pe", lambda e, pb=pb, j=j, kc=kc, x1_=x1_: e.transpose(
                            out=pb[:, j, :], in_=x1_[:, kc * 128:(kc + 1) * 128], identity=ident[:]),
                            reads=[x1_, ident], writes=[pb], wadd=(j > 0))
                    kb.op("act", lambda e, pb=pb, k4=k4: e.activation(out=xf[:, k4 * 4:(k4 + 1) * 4, :], in_=pb[:], func=AF.Copy),
                          reads=[pb], writes=[xf], wadd=(k4 > 0))
                    kb.op("dve", lambda e, k4=k4, tt=tt: e.tensor_copy(
                        out=x1Tb[:, k4 * 4:(k4 + 1) * 4, tt * 128:(tt + 1) * 128], in_=xf[:, k4 * 4:(k4 + 1) * 4, :]),
                        reads=[xf], writes=[x1Tb], wadd=not (tt == 0 and k4 == 0))
                for kc in range(16):
                    kb.op("pe", lambda e, kc=kc: e.matmul(prt[:, 0:16], lhsT=xf[:, kc, :], rhs=wr[:, kc, :], start=(kc == 0), stop=(kc == 15)),
                          reads=[xf, wr], writes=[prt], wadd=(kc > 0))
                gt_ = gate[tt % 2]
                route(kb, prt, brt, R, gt_)
                kb.dma("pool", gates[tok0:tok0 + 128, :], gt_[:], reads=[gt_])
                ti = tbk * 4 + tt
                kb.op("act", lambda e: e.activation(out=chb[:], in_=R["ch"][:], func=AF.Copy), reads=[R["ch"]], writes=[chb])
                kb.op("pe", lambda e: e.matmul(prt[:, 32:48], lhsT=lo[:, 0:128], rhs=chb[:], start=True, stop=True), reads=[lo, chb], writes=[prt])
                kb.op("pe", lambda e: e.matmul(prt[:, 48:64], lhsT=lo[:, 128:256], rhs=chb[:], start=True, stop=True), reads=[lo, chb], writes=[prt], wadd=True)
                if ti == 0:
                    kb.op("dve", lambda e: e.tensor_copy(out=R["pos"][:], in_=prt[:, 32:48]), reads=[prt], writes=[R["pos"]])
                    kb.op("dve", lambda e: e.tensor_copy(out=R["cntb"][:], in_=prt[:, 48:64]), reads=[prt], writes=[R["cntb"]])
                else:
                    kb.op("dve", lambda e: e.tensor_tensor(out=R["pos"][:], in0=prt[:, 32:48], in1=R["cntb"][:], op=ALU.add),
                          reads=[prt, R["cntb"]], writes=[R["pos"]])
                    kb.op("dve", lambda e: e.tensor_tensor(out=R["cntb"][:], in0=prt[:, 48:64], in1=R["cntb"][:], op=ALU.add),
                          reads=[prt, R["cntb"]], writes=[R["cntb"]])
                kb.op("dve", lambda e: e.tensor_scalar(out=R["ovf"][:], in0=R["pos"][:], scalar1=float(CAP), scalar2=1e7, op0=ALU.is_ge, op1=ALU.mult),
                      reads=[R["pos"]], writes=[R["ovf"]])
                kb.op("dve", lambda e: e.tensor_tensor(out=R["pos"][:], in0=R["pos"][:], in1=rowb[:], op=ALU.add), reads=[R["pos"], rowb], writes=[R["pos"]])
                kb.op("dve", lambda e: e.tensor_tensor(out=R["pos"][:], in0=R["pos"][:], in1=R["ovf"][:], op=ALU.add), reads=[R["pos"], R["ovf"]], writes=[R["pos"]])
                g_ = g12[tt % 2]
                ri_ = r12i[tt % 2]
                for c_, sel_ in ((0, "is1"), (1, "is2")):
                    kb.op("dve", lambda e, sel_=sel_: e.tensor_tensor(out=R["tmp"][:], in0=R[sel_][:], in1=R["pos"][:], op=ALU.mult),
                          reads=[R[sel_], R["pos"]], writes=[R["tmp"]])
                    kb.op("dve", lambda e, c_=c_: e.tensor_reduce(out=R["r12"][:, c_:c_ + 1], in_=R["tmp"][:], axis=AX.X, op=ALU.add),
                          reads=[R["tmp"]], writes=[R["r12"]], wadd=(c_ > 0))
                    kb.op("dve", lambda e, sel_=sel_, gt_=gt_: e.tensor_tensor(out=R["tmp"][:], in0=R[sel_][:], in1=gt_[:], op=ALU.mult),
                          reads=[R[sel_], gt_], writes=[R["tmp"]])
                    kb.op("dve", lambda e, c_=c_, g_=g_: e.tensor_reduce(out=g_[:, c_:c_ + 1], in_=R["tmp"][:], axis=AX.X, op=ALU.add),
                          reads=[R["tmp"]], writes=[g_], wadd=(c_ > 0))
                kb.op("dve", lambda e, ri_=ri_: e.tensor_copy(out=ri_[:], in_=R["r12"][:]), reads=[R["r12"]], writes=[ri_])
                tk_ = tk1[tt % 2]
                kb.op("dve", lambda e, tk_=tk_, ti=ti: e.tensor_copy(out=tk_[:], in_=tki[:, ti:ti + 1]), reads=[tki], writes=[tk_])
                for c_ in range(2):
                    rc_ = r1c[(tt % 2) * 2 + c_]
                    kb.op("dve", lambda e, rc_=rc_, c_=c_: e.tensor_copy(out=rc_[:], in_=R["r12"][:, c_:c_ + 1]), reads=[R["r12"]], writes=[rc_])
                    kb.idma(lst, tk_[:], rc_[:], True, 16 * CAP - 1, reads=[tk_, rc_, dl], writes=[dl], wadd=True)
                kb.dma("pool", ridx_d[tok0:tok0 + 128, :], ri_[:], reads=[ri_])
                kb.dma("pool", gsel_d[tok0:tok0 + 128, :], g_[:], reads=[g_])
                xb_ = x1b[tt % 2]
                kb.op("act", lambda e, xb_=xb_, x1_=x1_: e.activation(out=xb_[:], in_=x1_[:], func=AF.Copy), reads=[x1_], writes=[xb_])
                kb.dma("pool", x1b_d[tok0:tok0 + 128, :], xb_[:], reads=[xb_])
            for tt in range(4):
                phase3(tt)
            kb.dma("pool", x1T[:, tsl].rearrange("(k p) t -> p k t", p=128), x1Tb[:], reads=[x1Tb])
        kb.flush()


def route(kb, prt, brt, R, gate):
    def v3(b):
        return b[:].rearrange("p (g e) -> p g e", g=4)

    def bc(b):
        return b[:].unsqueeze(2).broadcast_to([128, 4, 4])
    D_ = "dve"
    kb.op("act", lambda e: e.activation(out=R["aff"][:], in_=prt[:, 0:16], func=AF.Sigmoid), reads=[prt], writes=[R["aff"]])
    kb.op(D_, lambda e: e.tensor_tensor(out=R["sel"][:], in0=R["aff"][:], in1=brt[:], op=ALU.add), reads=[R["aff"], brt], writes=[R["sel"]])
    kb.op(D_, lambda e: e.tensor_reduce(out=R["m1"][:], in_=v3(R["sel"]), axis=AX.X, op=ALU.max), reads=[R["sel"]], writes=[R["m1"]])
    kb.op(D_, lambda e: e.tensor_tensor(out=v3(R["eq"]), in0=v3(R["sel"]), in1=bc(R["m1"]), op=ALU.is_equal),
          reads=[R["sel"], R["m1"]], writes=[R["eq"]])
    kb.op(D_, lambda e: e.scalar_tensor_tensor(out=R["sel2"][:], in0=R["eq"][:], scalar=-1e9, in1=R["sel"][:], op0=ALU.mult, op1=ALU.add),
          reads=[R["eq"], R["sel"]], writes=[R["sel2"]])
    kb.op(D_, lambda e: e.tensor_reduce(out=R["m2"][:], in_=v3(R["sel2"]), axis=AX.X, op=ALU.max), reads=[R["sel2"]], writes=[R["m2"]])
    kb.op(D_, lambda e: e.tensor_tensor(out=R["m1"][:], in0=R["m1"][:], in1=R["m2"][:], op=ALU.add), reads=[R["m1"], R["m2"]], writes=[R["m1"]])
    kb.op(D_, lambda e: e.tensor_reduce(out=R["gm"][:], in_=R["m1"][:], axis=AX.X, op=ALU.max), reads=[R["m1"]], writes=[R["gm"]])
    kb.op(D_, lambda e: e.tensor_scalar(out=R["m2"][:], in0=R["m1"][:], scalar1=R["gm"][:, 0:1], scalar2=None, op0=ALU.is_equal),
          reads=[R["m1"], R["gm"]], writes=[R["m2"]])
    kb.op(D_, lambda e: e.tensor_scalar(out=R["m2"][:], in0=R["m2"][:], scalar1=1e9, scalar2=-1e9, op0=ALU.mult, op1=ALU.add),
          reads=[R["m2"]], writes=[R["m2"]])
    kb.op(D_, lambda e: e.tensor_tensor(out=v3(R["sel2"]), in0=v3(R["sel"]), in1=bc(R["m2"]), op=ALU.add),
          reads=[R["sel"], R["m2"]], writes=[R["sel2"]])
    kb.op(D_, lambda e: e.tensor_reduce(out=R["t1"][:], in_=R["sel2"][:], axis=AX.X, op=ALU.max), reads=[R["sel2"]], writes=[R["t1"]])
    kb.op(D_, lambda e: e.tensor_scalar(out=R["is1"][:], in0=R["sel2"][:], scalar1=R["t1"][:, 0:1], scalar2=None, op0=ALU.is_equal),
          reads=[R["sel2"], R["t1"]], writes=[R["is1"]])
    kb.op(D_, lambda e: e.scalar_tensor_tensor(out=R["sel2"][:], in0=R["is1"][:], scalar=-3e9, in1=R["sel2"][:], op0=ALU.mult, op1=ALU.add),
          reads=[R["is1"], R["sel2"]], writes=[R["sel2"]])
    kb.op(D_, lambda e: e.tensor_reduce(out=R["t2"][:], in_=R["sel2"][:], axis=AX.X, op=ALU.max), reads=[R["sel2"]], writes=[R["t2"]])
    kb.op(D_, lambda e: e.tensor_scalar(out=R["is2"][:], in0=R["sel2"][:], scalar1=R["t2"][:, 0:1], scalar2=None, op0=ALU.is_equal),
          reads=[R["sel2"], R["t2"]], writes=[R["is2"]])
    kb.op(D_, lambda e: e.tensor_tensor(out=R["ch"][:], in0=R["is2"][:], in1=R["is1"][:], op=ALU.add), reads=[R["is2"], R["is1"]], writes=[R["ch"]])
    kb.op(D_, lambda e: e.tensor_tensor(out=R["eq"][:], in0=R["ch"][:], in1=R["aff"][:], op=ALU.mult), reads=[R["ch"], R["aff"]], writes=[R["eq"]])
    kb.op(D_, lambda e: e.tensor_reduce(out=R["den"][:], in_=R["eq"][:], axis=AX.X, op=ALU.add), reads=[R["eq"]], writes=[R["den"]])
    kb.op(D_, lambda e: e.reciprocal(out=R["den"][:], in_=R["den"][:]), reads=[R["den"]], writes=[R["den"]])
    kb.op(D_, lambda e: e.tensor_scalar(out=gate[:], in0=R["eq"][:], scalar1=R["den"][:, 0:1], scalar2=None, op0=ALU.mult),
          reads=[R["eq"], R["den"]], writes=[gate])


def st_moe(kb, wb, spl, cf32, cbf, xcur1, x1T, pin, xout, lst, ridx_d, gsel_d, x1b_d, ybuf):
    NBLK = CAP // 512
    with ExitStack() as st:
        ident = kb.sb(st, "ident", [128, 128], F32)
        kb.dma("sp", ident[:], cf32[:, CF_ID:CF_ID + 128], writes=[ident])
        identb = kb.sb(st, "identb", [128, 128], BF16)
        kb.dma("sp", identb[:], cbf[:, CB_ID:CB_ID + 128], writes=[identb])
        lnp = kb.sb(st, "lnp", [128, 2, 2048], F32)
        kb.dma("sp", lnp[:, 0, :], spl[:, SP["LNG"][0] + 2048:SP["LNG"][0] + 4096], writes=[lnp])
        kb.dma("sp", lnp[:, 1, :], spl[:, SP["LNB"][0] + 2048:SP["LNB"][0] + 4096], writes=[lnp], wadd=True)
        x1Tbs = [kb.sb(st, "x1Tb%d" % i, [128, 16, 512], BF16) for i in range(2)]
        acc = kb.sb(st, "acc", [128, 4, 2048], F32)
        hT = kb.sb(st, "hT", [128, 8, 512], BF16)
        wsl = [kb.sb(st, "wsl%d" % i, [128, 8192], BF16) for i in range(3)]
        wpl = [kb.sb(st, "wpl%d" % i, [128, 2, 512], BF16) for i in range(2)]
        sg = [kb.sb(st, "sg%d" % i, [128, 512], F32) for i in range(2)]
        pts = [kb.sb(st, "pt_%d" % i, [128, 4, 256], F32) for i in range(2)]
        pT = kb.sb(st, "pT", [128, 2, 512], BF16)
        x1t = [kb.sb(st, "x1t%d" % i, [128, 2048], F32) for i in range(2)]
        xs = [kb.sb(st, "xs%d" % i, [128, 2048], BF16) for i in range(4)]
        yg = [kb.sb(st, "yg%d" % i, [128, 2048], F32) for i in range(2)]
        idxt = [kb.sb(st, "idxt%d" % i, [128, 1], I32) for i in range(4)]
        ris = [kb.sb(st, "ri%d" % i, [128, 4, 2], I32) for i in range(2)]
        ric = [kb.sb(st, "ric%d" % i, [128, 1], I32) for i in range(2)]
        gss = [kb.sb(st, "gs%d" % i, [128, 4, 2], F32) for i in range(2)]
        bs = kb.sb(st, "bs", [128, 4, 6], F32)
        mv = kb.sb(st, "mv", [128, 2], F32)
        rs = kb.sb(st, "rs", [128, 1], F32)
        pg = [kb.ps(st, "pg%d" % i, [128, 512], F32) for i in range(4)]
        py = [kb.ps(st, "py%d" % i, [128, 512], F32) for i in range(2)]
        ptr = kb.ps(st, "ptr", [128, 4, 128], F32)
        ptb = kb.ps(st, "ptb", [128, 8, 128], BF16)
        dy = Buf("ybuf", None)
        wc = 0
        pc = 0
        yc_ = 0
        sc = 0
        ev = 0
        gcnt = 0
        for i in range(4):
            kb.op("pool", lambda e, i=i: e.memset(xs[i][:], 0.0), writes=[xs[i]])
        for e_ in range(16):
            for blk in range(NBLK):
                x1Tb = x1Tbs[(e_ * NBLK + blk) % 2]
                for tt in range(4):
                    row0 = e_ * CAP + blk * 512 + tt * 128
                    it_ = idxt[gcnt % 4]
                    xs_ = xs[gcnt % 4]
                    gcnt += 1
                    kb.dma("sp", it_[:], lst[row0:row0 + 128, :], writes=[it_])
                    kb.idma(xs_[:], x1b_d, it_[:, 0:1], False, T - 1, reads=[it_], writes=[xs_], wadd=True)
                    for k8 in range(2):
                        for j in range(8):
                            kc = k8 * 8 + j
                            kb.op("pe", lambda e, j=j, kc=kc, xs_=xs_: e.transpose(
                                out=ptb[:, j, :], in_=xs_[:, kc * 128:(kc + 1) * 128], identity=identb[:]),
                                reads=[xs_, identb], writes=[ptb], wadd=(j > 0))
                        oap = x1Tb[:, k8 * 8:(k8 + 1) * 8, tt * 128:(tt + 1) * 128]
                        first = (tt == 0 and k8 == 0)
                        if ev % 2 == 0:
                            kb.op("act", lambda e, oap=oap: e.activation(out=oap, in_=ptb[:], func=AF.Copy),
                                  reads=[ptb], writes=[x1Tb], wadd=not first)
                        else:
                            kb.op("dve", lambda e, oap=oap: e.tensor_copy(out=oap, in_=ptb[:]),
                                  reads=[ptb], writes=[x1Tb], wadd=not first)
                        ev += 1
                for jc in range(8):
                    w_ = wsl[wc % 3]
                    wc += 1
                    wv = w_[:, 0:4096].rearrange("p (a k j) -> p a k j", a=2, k=16)
                    kb.dma("sp", wv, wview(wb, "GU", (e_ * 8 + jc, 128 * 2 * 16 * 128), "(p a k j) -> p a k j", p=128, a=2, k=16), writes=[w_])
                    pgg, pgu = pg[pc % 4], pg[(pc + 1) % 4]
                    pc += 2
                    for (pp, ai) in ((pgg, 0), (pgu, 1)):
                        for kc in range(16):
                            kb.op("pe", lambda e, pp=pp, ai=ai, kc=kc, wv=wv, x1Tb=x1Tb: e.matmul(
                                pp[:], lhsT=wv[:, ai, kc, :], rhs=x1Tb[:, kc, :], start=(kc == 0), stop=(kc == 15)),
                                reads=[w_, x1Tb], writes=[pp], wadd=(kc > 0))
                    s_ = sg[sc % 2]
                    sc += 1
                    kb.op("act", lambda e, s_=s_, pgg=pgg: e.activation(out=s_[:], in_=pgg[:], func=AF.Silu), reads=[pgg], writes=[s_])
                    kb.op("dve", lambda e, s_=s_, pgu=pgu, jc=jc: e.tensor_tensor(out=hT[:, jc, :], in0=s_[:], in1=pgu[:], op=ALU.mult),
                          reads=[s_, pgu], writes=[hT], wadd=(jc > 0))
                for n4 in range(4):
                    w_ = wsl[wc % 3]
                    wc += 1
                    wv = w_[:, 0:4096].rearrange("p (j n) -> p j n", j=8)
                    kb.dma("sp", wv, wview(wb, "DN", (e_ * 4 + n4, 128 * 8 * 512), "(p j n) -> p j n", p=128, j=8), writes=[w_])
                    for tt in range(4):
                        p_ = py[yc_ % 2]
                        yc_ += 1
                        for jc in range(8):
                            kb.op("pe", lambda e, p_=p_, jc=jc, tt=tt, wv=wv: e.matmul(
                                p_[:], lhsT=hT[:, jc, tt * 128:(tt + 1) * 128], rhs=wv[:, jc, :], start=(jc == 0), stop=(jc == 7)),
                                reads=[hT, w_], writes=[p_], wadd=(jc > 0))
                        asl = acc[:, tt, n4 * 512:(n4 + 1) * 512]
                        firstw = (n4 == 0 and tt == 0)
                        if ev % 2 == 0:
                            kb.op("act", lambda e, p_=p_, asl=asl: e.activation(out=asl, in_=p_[:], func=AF.Copy),
                                  reads=[p_], writes=[acc], wadd=not firstw)
                        else:
                            kb.op("dve", lambda e, p_=p_, asl=asl: e.tensor_copy(out=asl, in_=p_[:]),
                                  reads=[p_], writes=[acc], wadd=not firstw)
                        ev += 1
                r0 = e_ * CAP + blk * 512
                kb.dma("act", ybuf[r0:r0 + 512, :].rearrange("(a p) d -> p a d", p=128), acc[:], reads=[acc], writes=[dy], wadd=True, dbuf=acc)
        for tbk in range(8):
            tsl = slice(tbk * 512, (tbk + 1) * 512)
            x1Tb, ri, gs, pt_ = x1Tbs[tbk % 2], ris[tbk % 2], gss[tbk % 2], pts[tbk % 2]
            kb.dma("sp", x1Tb[:], x1T[:, tsl].rearrange("(k p) t -> p k t", p=128), writes=[x1Tb])
            kb.dma("sp", ri[:], ridx_d[tsl, :].rearrange("(a p) c -> p a c", p=128), writes=[ri])
            kb.dma("sp", gs[:], gsel_d[tsl, :].rearrange("(a p) c -> p a c", p=128), writes=[gs])
            kb.dma("sp", pt_[:], pin[tsl, :].rearrange("(a p) c -> p a c", p=128), writes=[pt_])
            for tt in range(4):
                for c_ in range(2):
                    y_ = yg[c_]
                    ic_ = ric[c_]
                    kb.op("pool", lambda e, ic_=ic_, tt=tt, c_=c_, ri=ri: e.tensor_copy(out=ic_[:], in_=ri[:, tt, c_:c_ + 1]), reads=[ri], writes=[ic_])
                    kb.idma(y_[:], ybuf, ic_[:], False, 16 * CAP - 1, reads=[ic_, dy], writes=[y_])
                    if c_ == 0:
                        kb.op("dve", lambda e, y_=y_, tt=tt, gs=gs: e.tensor_scalar(
                            out=acc[:, tt, :], in0=y_[:], scalar1=gs[:, tt, 0:1], scalar2=None, op0=ALU.mult),
                            reads=[y_, gs], writes=[acc], wadd=(tt > 0))
                    else:
                        kb.op("dve", lambda e, y_=y_, tt=tt, gs=gs: e.scalar_tensor_tensor(
                            out=acc[:, tt, :], in0=y_[:], scalar=gs[:, tt, 1:2], in1=acc[:, tt, :], op0=ALU.mult, op1=ALU.add),
                            reads=[y_, gs, acc], writes=[acc], wadd=True)
            for tt in range(4):
                for kc in range(2):
                    kb.op("pe", lambda e, tt=tt, kc=kc, pt_=pt_: e.transpose(out=ptr[:, kc, :], in_=pt_[:, tt, kc * 128:(kc + 1) * 128], identity=ident[:]),
                          reads=[pt_, ident], writes=[ptr], wadd=(kc > 0))
                kb.op("act", lambda e, tt=tt: e.activation(out=pT[:, :, tt * 128:(tt + 1) * 128], in_=ptr[:, 0:2, :], func=AF.Copy),
                      reads=[ptr], writes=[pT], wadd=(tt > 0))
            for n4 in range(4):
                w_ = wsl[wc % 3]
                wc += 1
                wv = w_[:].rearrange("p (k n) -> p k n", k=16)
                kb.dma("sp", wv, wview(wb, "PG", (n4, 128 * 16 * 512), "(p k n) -> p k n", p=128, k=16), writes=[w_])
                wp_ = wpl[n4 % 2]
                kb.dma("sp", wp_[:], wview(wb, "PLE", (n4, 128 * 2 * 512), "(p k n) -> p k n", p=128, k=2), writes=[wp_])
                for tt in range(4):
                    pa1, pa2 = pg[pc % 4], pg[(pc + 1) % 4]
                    pc += 2
                    for kc in range(16):
                        kb.op("pe", lambda e, pa1=pa1, kc=kc, tt=tt, wv=wv, x1Tb=x1Tb: e.matmul(
                            pa1[:], lhsT=x1Tb[:, kc, tt * 128:(tt + 1) * 128], rhs=wv[:, kc, :], start=(kc == 0), stop=(kc == 15)),
                            reads=[x1Tb, w_], writes=[pa1], wadd=(kc > 0))
                    for kc in range(2):
                        kb.op("pe", lambda e, pa2=pa2, kc=kc, tt=tt, wp_=wp_: e.matmul(
                            pa2[:], lhsT=pT[:, kc, tt * 128:(tt + 1) * 128], rhs=wp_[:, kc, :], start=(kc == 0), stop=(kc == 1)),
                            reads=[pT, wp_], writes=[pa2], wadd=(kc > 0))
                    s_ = sg[sc % 2]
                    sc += 1
                    kb.op("act", lambda e, s_=s_, pa1=pa1: e.activation(out=s_[:], in_=pa1[:], func=AF.Sigmoid), reads=[pa1], writes=[s_])
                    kb.op("dve", lambda e, s_=s_, pa2=pa2: e.tensor_tensor(out=s_[:], in0=s_[:], in1=pa2[:], op=ALU.mult),
                          reads=[s_, pa2], writes=[s_])
                    asl = acc[:, tt, n4 * 512:(n4 + 1) * 512]
                    kb.op("dve", lambda e, s_=s_, asl=asl: e.tensor_tensor(out=asl, in0=asl, in1=s_[:], op=ALU.add),
                          reads=[s_, acc], writes=[acc], wadd=True)
            for tt in range(4):
                x_ = x1t[tt % 2]
                tok0 = tbk * 512 + tt * 128
                kb.dma("sp", x_[:], xcur1[tok0:tok0 + 128, :], writes=[x_])
                kb.op("dve", lambda e, x_=x_, tt=tt: e.scalar_tensor_tensor(
                    out=acc[:, tt, :], in0=x_[:], scalar=ALPHA, in1=acc[:, tt, :], op0=ALU.mult, op1=ALU.add),
                    reads=[x_, acc], writes=[acc], wadd=True)
                ln_tile(kb, acc, (slice(None), tt, slice(None)), lnp, x_, (slice(None), slice(None)), (bs, mv, rs))
                kb.dma("pool", xout[tok0:tok0 + 128, :], x_[:], reads=[x_])
        kb.flush()


def prep_layer_weights(inp, l):
    f32 = np.float32
    out = np.empty(NW, f32)

    def put(name, arr):
        a = np.ascontiguousarray(arr, dtype=f32).reshape(-1)
        out[OFF[name]:OFF[name] + a.size] = a

    perm = np.concatenate([np.arange(0, 7680), np.arange(7728, 13872), np.arange(7680, 7728)])
    w = inp["w_in"][l][:, perm]
    put("IN", w[:, :13824].reshape(16, 128, 108, 128).transpose(2, 1, 0, 3))
    put("INL", w[:, 13824:].reshape(16, 128, 48).transpose(1, 0, 2))
    put("BR", inp["w_branch"][l].reshape(3, 8, 128, 16, 128).transpose(3, 2, 0, 1, 4))
    put("OUT", inp["w_out"][l].reshape(16, 128, 4, 512).transpose(2, 1, 0, 3))
    put("PG", inp["w_ple_gate"][l].reshape(16, 128, 4, 512).transpose(2, 1, 0, 3))
    put("PLE", inp["w_ple"][l].reshape(2, 128, 4, 512).transpose(2, 1, 0, 3))
    put("GU", inp["w_gate_up"][l].reshape(16, 16, 128, 2, 8, 128).transpose(0, 4, 2, 3, 1, 5))
    put("DN", inp["w_down"][l].reshape(16, 8, 128, 4, 512).transpose(0, 3, 2, 1, 4))
    put("LRU", np.stack([inp["lru_wa"][l], inp["lru_wx"][l]]).transpose(2, 0, 1, 3))
    put("W1", inp["phi_w1"][l].reshape(2, 32, 64, 128).transpose(2, 0, 1, 3))
    put("W2", inp["phi_w2"][l].transpose(1, 0, 2))
    put("POS", inp["cmp_pos"][l].transpose(2, 0, 1))
    return out


def prep_small(inp, l):
    f32 = np.float32
    sp = np.zeros((128, NS), f32)

    def put(name, arr):
        o, n = SP[name]
        sp[:, o:o + n] = np.asarray(arr, f32).reshape(128, n)

    put("CW", inp["conv_a_w"][l].reshape(3, 8, 128).transpose(2, 1, 0))
    put("CB", inp["conv_a_b"][l].reshape(8, 128).T)
    put("LW", inp["lru_conv_w"][l].reshape(4, 8, 128).transpose(2, 1, 0))
    put("LB", inp["lru_conv_b"][l].reshape(8, 128).T)
    put("BA", inp["lru_ba"][l].T)
    put("BX", inp["lru_bx"][l].T)
    put("LAM", inp["lru_lam"][l].reshape(8, 128).T)
    put("B1", inp["phi_b1"][l].T)
    put("WR", inp["w_router"].reshape(16, 128, 16).transpose(1, 0, 2))
    put("BR", np.broadcast_to(inp["b_router"][None, :], (128, 16)))
    put("LNG", np.broadcast_to(inp["ln_g"][l].reshape(1, 4096), (128, 4096)))
    put("LNB", np.broadcast_to(inp["ln_b"][l].reshape(1, 4096), (128, 4096)))
    return sp


def const_tables():
    f32 = np.float32
    cb = np.zeros((128, NCB), f32)
    k = np.arange(128)[:, None]
    qq = np.arange(128)[None, :]
    cb[:, CB_ID:CB_ID + 128] = np.eye(128)
    cb[:, CB_CAUS:CB_CAUS + 128] = np.where(k > qq, NEGB, 0.0)
    cb[:, CB_TAIL:CB_TAIL + 128] = np.where(k <= qq, NEGB, 0.0)
    for b in range(32):
        E = np.zeros((128, 128), f32)
        E[2 * b, 0:64] = 1.0
        E[2 * b + 1, 64:128] = 1.0
        cb[:, CB_E + b * 128:CB_E + (b + 1) * 128] = E
    c_start = np.arange(255) * 16
    s_start = np.arange(64) * 64
    ov = np.clip(np.minimum(c_start[:, None] + 32, s_start[None, :] + 64) - np.maximum(c_start[:, None], s_start[None, :]), 0, None) / 32.0
    cm = np.zeros((256, 64), f32)
    cm[:255] = ov
    for nt in range(2):
        cb[:, CB_CM + nt * 64:CB_CM + (nt + 1) * 64] = cm[nt * 128:(nt + 1) * 128]
    for a in range(32):
        for nt in range(2):
            n = nt * 128 + np.arange(128)[:, None]
            t = a * 128 + np.arange(128)[None, :]
            vis = (n <= 254) & (16 * n + 31 <= t)
            cb[:, CB_MASK + (a * 2 + nt) * 128:CB_MASK + (a * 2 + nt + 1) * 128] = np.where(vis, 0.0, NEGB)
    cf = np.zeros((128, NCF), f32)
    cf[:, CF_ID:CF_ID + 128] = np.eye(128)
    tl = np.arange(128)[:, None]
    rel = np.arange(128)[None, :] - 63
    curoff = (tl >= 64).astype(np.int64)
    cf[:, CF_WC:CF_WC + 128] = (rel < curoff)
    cf[:, CF_WF:CF_WF + 128] = np.where(rel < curoff, 0.0, np.where(rel == curoff, 1e9, -1e9))
    cf[:, CF_RB:CF_RB + 16] = (np.arange(16) * CAP)[None, :]
    cb[:, CB_L:CB_L + 128] = (k < qq)
    cb[:, CB_ONES:CB_ONES + 128] = 1.0
    slopes = 2.0 ** (-8.0 * np.arange(1, 17) / 16)
    hi = slopes.astype(f32).astype(BF).astype(np.float64)
    lo = (slopes - hi).astype(f32).astype(BF).astype(np.float64)
    pos = np.arange(4096)
    kaug = np.zeros((5, 4, 4096), f32)
    kaug[0] = kaug[1] = (pos // 128)[None, :]
    kaug[2] = kaug[3] = (pos % 128)[None, :]
    kaug[4] = 1.0
    cpos = 16 * np.arange(256) + 31
    kaugc = np.zeros((5, 4, 256), f32)
    kaugc[0] = kaugc[1] = (cpos // 128)[None, :]
    kaugc[2] = kaugc[3] = (cpos % 128)[None, :]
    kaugc[4] = 1.0
    qaug = np.zeros((5, 16, 4096), f32)
    tref = (pos // 128) * 128 + 64
    qaug[0] = (128 * hi)[:, None]
    qaug[1] = (128 * lo)[:, None]
    qaug[2] = hi[:, None]
    qaug[3] = lo[:, None]
    qaug[4] = -(hi + lo)[:, None] * tref[None, :]
    tokid = (np.arange(32)[None, :] * 128 + np.arange(128)[:, None]).astype(np.int32)
    oob = np.full((16 * CAP, 1), 1 << 30, np.int32)
    return dict(tokid=tokid, oob=oob, cbf=cb.astype(BF), cf32=cf, kaug=kaug.astype(BF), kaugc=kaugc.astype(BF), qaug=qaug.astype(BF))


def build_nc(n_layers=DEPTH, stages=None, dbg=False):
    nc = bass.Bass("TRN2", target_bir_lowering=False)

    def din(name, shape, dt):
        return nc.dram_tensor(name, shape, dt, kind="ExternalInput").ap()

    def dscr(name, shape, dt):
        return nc.dram_tensor(name, shape, dt, kind=("ExternalOutput" if dbg else "Internal")).ap()

    x = din("x", [T, D], F32)
    pin = din("pin", [n_layers * T, 256], F32)
    wl = din("wl", [n_layers * NW if (stages is None or "cast" in stages) else 128], F32)
    sp = din("sp", [n_layers * 128, NS], F32)
    cbf = din("cbf", [128, NCB], BF16)
    cf32 = din("cf32", [128, NCF], F32)
    kaug = din("kaug", [5, 4, T], BF16)
    kaugc = din("kaugc", [5, 4, 256], BF16)
    qaug = din("qaug", [5, 16, T], BF16)
    tokid = din("tokid", [128, 32], I32)
    oob = din("oob", [16 * CAP, 1], I32)
    y = nc.dram_tensor("y", [T, D], F32, kind="ExternalOutput").ap()
    wb = [nc.dram_tensor("wb%d" % i, [s1 - s0], BF16, kind="Internal").ap() for i, (s0, s1) in enumerate(SEGS)]
    zT = dscr("zT", [NZ, T], BF16)
    yT = dscr("yT", [3072, T], BF16)
    xcur1 = dscr("xcur1", [T, D], F32)
    x1T = dscr("x1T", [D, T], BF16)
    gates = dscr("gates", [T, 16], F32)
    xmid = nc.dram_tensor("xmid", [T, D], F32, kind="Internal").ap()
    lst = nc.dram_tensor("lst", [16 * CAP, 1], I32, kind="Internal").ap()
    ridx_d = nc.dram_tensor("ridx", [T, 2], I32, kind="Internal").ap()
    gsel_d = nc.dram_tensor("gsel", [T, 2], F32, kind="Internal").ap()
    x1b_d = nc.dram_tensor("x1b", [T, D], BF16, kind="Internal").ap()
    ybuf = nc.dram_tensor("ybuf", [16 * CAP, D], F32, kind="Internal").ap()
    with ExitStack() as es:
        kb = KB(nc, es)
        for l in range(n_layers):
            xin = x if l == 0 else xmid
            xout = y if l == n_layers - 1 else xmid
            spl = sp[l * 128:(l + 1) * 128, :]

            def on(s):
                return stages is None or s in stages
            wsrc = wl[l * NW:(l + 1) * NW] if on("cast") else None
            BG = True
            if on("cast"):
                st_cast(kb, cast_jobs(wsrc, wb, [0] if BG else [0, 1, 2, 3]))
            if on("inproj"):
                st_inproj(kb, xin, wb, zT, cf32, bg_jobs=(cast_jobs(wsrc, wb, [1, 2, 3]) if (BG and on("cast")) else ()))
            if on("conv"):
                st_conv(kb, zT, yT, spl)
            if on("lru"):
                st_lru(kb, zT, yT, spl, wb)
            if on("nsa"):
                st_nsa(kb, zT, yT, wb, spl, cbf, cf32, kaug, kaugc, qaug)
            if on("mix"):
                st_mix(kb, xin, zT, yT, wb, spl, cf32, xcur1, x1T, gates, cbf, tokid, oob, lst, ridx_d, gsel_d, x1b_d)
            if on("moe"):
                st_moe(kb, wb, spl, cf32, cbf, xcur1, x1T, pin[l * T:(l + 1) * T, :], xout, lst, ridx_d, gsel_d, x1b_d, ybuf)
    return nc


_NC_CACHE = {}


def kernel(**inputs):
    inp = {k: np.asarray(v) for k, v in inputs.items()}
    B = inp["x"].shape[0]
    wl = np.concatenate([prep_layer_weights(inp, l) for l in range(DEPTH)])
    sp = np.concatenate([prep_small(inp, l) for l in range(DEPTH)], axis=0)
    ct = const_tables()
    if "nc" not in _NC_CACHE:
        _NC_CACHE["nc"] = build_nc()
    nc = _NC_CACHE["nc"]
    in_maps = []
    for b in range(B):
        m = dict(x=np.ascontiguousarray(inp["x"][b]),
                 pin=np.ascontiguousarray(inp["p"][:, b]).reshape(DEPTH * T, 256),
                 wl=wl, sp=sp)
        m.update(ct)
        in_maps.append(m)
    res = run_bass_kernel_spmd(nc, in_maps, core_ids=list(range(B)))
    return np.stack([np.asarray(r["y"], dtype=np.float32) for r in res.results], axis=0)
```

```python
import numpy as np
from contextlib import ExitStack
import concourse.bass as bass
import concourse.mybir as mybir
from concourse.bass_utils import run_bass_kernel_spmd

F32 = mybir.dt.float32
BF16 = mybir.dt.bfloat16
AF = mybir.ActivationFunctionType
ALU = mybir.AluOpType
AX = mybir.AxisListType


class Buf:
    def __init__(self, name, t):
        self.name = name
        self.t = t
        self.w = []
        self.r = []
        self.r_old = []
        self.dsem = None

    def __getitem__(self, idx):
        return self.t[idx]


class KB:
    ENG = ("pe", "act", "dve", "pool", "sp")

    def __init__(self, nc, es):
        self.nc = nc
        self.es = es
        self.sem = {}
        self.count = {}
        self.known = {}
        for e in self.ENG:
            self.sem[e] = es.enter_context(nc.semaphore("sem_" + e))
            self.count[e] = 0
        self.dsem_pool = [es.enter_context(nc.semaphore("dsem%d" % i)) for i in range(80)]
        self.dsem_count = {}
        self.dsem_next = 0
        self.rec = {e: [] for e in self.ENG}
        self.known = {e: {} for e in self.ENG}
        self.stage_dsems = []
        self.uid = 0

    def sb(self, st, name, shape, dt):
        self.uid += 1
        t = st.enter_context(self.nc.sbuf_tensor("%s_%d" % (name, self.uid), list(shape), dt))
        return Buf(name, t)

    def ps(self, st, name, shape, dt):
        self.uid += 1
        t = st.enter_context(self.nc.psum_tensor("%s_%d" % (name, self.uid), list(shape), dt))
        return Buf(name, t)

    def _dsem(self, b):
        if b.dsem is None:
            s = self.dsem_pool[self.dsem_next % len(self.dsem_pool)]
            self.dsem_next += 1
            b.dsem = s
            self.dsem_count.setdefault(id(s), [s, 0])
        return b.dsem

    def _waits(self, eng, reads, writes, wadd):
        toks = []
        for b in reads:
            toks += b.w
        for b in writes:
            if not wadd:
                toks += b.w
            else:
                toks += b.r_old
            toks += b.r
        best = {}
        for (s, v, e) in toks:
            if eng == "pe" and e == "pe":
                continue
            k = id(s)
            if k not in best or best[k][1] < v:
                best[k] = (s, v)
        out = []
        kn = self.known[eng]
        for k, (s, v) in best.items():
            if kn.get(k, 0) >= v:
                continue
            kn[k] = v
            out.append((s, v))
        return out

    def op(self, eng, fn, reads=(), writes=(), wadd=False):
        waits = self._waits(eng, reads, writes, wadd)
        self.count[eng] += 1
        tok = (self.sem[eng], self.count[eng], eng)
        self.rec[eng].append((waits, fn, (self.sem[eng], 1)))
        for b in reads:
            b.r.append(tok)
        for b in writes:
            if wadd:
                b.w.append(tok)
            else:
                b.w = [tok]
                b.r_old = b.r
                b.r = []
        return tok

    def dma(self, q, out_ap, in_ap, reads=(), writes=(), wadd=False, dbuf=None, **kw):
        waits = self._waits(q, reads, writes, wadd)
        if dbuf is None:
            dbuf = writes[0] if writes else reads[0]
        s = self._dsem(dbuf)
        ent = self.dsem_count[id(s)]
        ent[1] += 16
        tok = (s, ent[1], "dma")

        def fn(e, out_ap=out_ap, in_ap=in_ap, kw=kw):
            try:
                return e.dma_start(out=out_ap, in_=in_ap, **kw)
            except Exception:
                print("DMA FAIL", out_ap, in_ap)
                raise

        self.rec[q].append((waits, fn, (s, 16)))
        for b in reads:
            b.r.append(tok)
        for b in writes:
            if wadd:
                b.w.append(tok)
            else:
                b.w = [tok]
                b.r_old = b.r
                b.r = []
        return tok

    def idma(self, out_ap, in_ap, idx_ap, scatter, bound, reads=(), writes=(), wadd=False, dbuf=None):
        waits = self._waits("pool", reads, writes, wadd)
        if dbuf is None:
            dbuf = writes[0]
        s = self._dsem(dbuf)
        ent = self.dsem_count[id(s)]
        ent[1] += 16
        tok = (s, ent[1], "dma")

        def fn(e):
            off = bass.IndirectOffsetOnAxis(ap=idx_ap, axis=0)
            if not hasattr(self, "bregs"):
                self.bregs = {}
            if bound not in self.bregs:
                r = e.alloc_register("bnd%d" % bound)
                e.reg_mov(r, bound)
                self.bregs[bound] = r
            breg = self.bregs[bound]
            if scatter:
                return e.indirect_dma_start(out=out_ap, out_offset=off, in_=in_ap, in_offset=None,
                                            bounds_check=breg, oob_is_err=False)
            return e.indirect_dma_start(out=out_ap, out_offset=None, in_=in_ap, in_offset=off,
                                        bounds_check=breg, oob_is_err=False)

        self.rec["pool"].append((waits, fn, (s, 16)))
        for b in reads:
            b.r.append(tok)
        for b in writes:
            if wadd:
                b.w.append(tok)
            else:
                b.w = [tok]
                b.r_old = b.r
                b.r = []
        return tok

    def flush(self, final=False):
        nc = self.nc
        tails = {}
        for e in self.ENG:
            w = []
            for f in self.ENG:
                if f != e and self.count[f] > self.known[e].get(id(self.sem[f]), 0):
                    w.append((self.sem[f], self.count[f]))
                    self.known[e][id(self.sem[f])] = self.count[f]
            for k, (s, v) in self.dsem_count.items():
                if v > self.known[e].get(k, 0):
                    w.append((s, v))
                    self.known[e][k] = v
            tails[e] = w
        rec = self.rec
        self.rec = {e: [] for e in self.ENG}
        assert self.dsem_next <= len(self.dsem_pool), self.dsem_next
        self.dsem_next = 0

        def replay(eng_obj, name):
            for (waits, fn, inc) in rec[name]:
                for (s, v) in waits:
                    eng_obj.wait_ge(s, v)
                ins = fn(eng_obj)
                ins.then_inc(inc[0], inc[1])
            for (s, v) in tails[name]:
                eng_obj.wait_ge(s, v)

        with nc.Block() as block:
            @block.tensor
            def _(e):
                replay(e, "pe")

            @block.scalar
            def _(e):
                replay(e, "act")

            @block.vector
            def _(e):
                replay(e, "dve")

            @block.gpsimd
            def _(e):
                replay(e, "pool")

            @block.sync
            def _(e):
                replay(e, "sp")
import ml_dtypes

BF = ml_dtypes.bfloat16
T = 4096
D = 2048
DEPTH = 2
NZ = 13872
ALPHA = (2 * DEPTH) ** 0.25
LN_EPS = 1e-5
ROW = dict(a_in=0, a_b=1024, a_c=2048, r_gate=3072, r_in=4096, q=5120, kc=6144, vc=6400,
           ks=6656, vs=6912, kw=7168, vw=7424, mg=7680, ng=13824)
NEGB = -30000.0

_sizes = [("IN", 108 * 128 * 16 * 128), ("INL", 128 * 16 * 48), ("BR", 16 * 128 * 3 * 8 * 128),
          ("OUT", 4 * 128 * 16 * 512), ("PG", 4 * 128 * 16 * 512), ("PLE", 4 * 128 * 2 * 512),
          ("GU", 16 * 8 * 128 * 2 * 16 * 128), ("DN", 16 * 4 * 128 * 8 * 512),
          ("LRU", 128 * 2 * 8 * 128), ("W1", 64 * 2 * 32 * 128), ("W2", 128 * 2 * 64), ("POS", 64 * 2 * 32)]
OFF = {}
_o = 0
for _n, _s in _sizes:
    OFF[_n] = _o
    _o += _s
NW = _o
assert NW % 128 == 0

_sp = [("CW", 24), ("CB", 8), ("LW", 32), ("LB", 8), ("BA", 8), ("BX", 8), ("LAM", 8), ("B1", 2),
       ("WR", 256), ("BR", 16), ("LNG", 4096), ("LNB", 4096)]
SP = {}
_o = 0
for _n, _s in _sp:
    SP[_n] = (_o, _s)
    _o += _s
NS = _o

CB_ID, CB_CAUS, CB_TAIL, CB_E, CB_CM, CB_MASK = 0, 128, 256, 384, 384 + 4096, 384 + 4096 + 128
CB_L = CB_MASK + 32 * 2 * 128
CB_ONES = CB_L + 128
NCB = CB_ONES + 128
CAP = 1024
I32 = mybir.dt.int32
CF_ID, CF_WC, CF_WF, CF_RB = 0, 128, 256, 384
NCF = 400


SEGS = [(0, OFF["BR"]), (OFF["BR"], OFF["GU"]), (OFF["GU"], OFF["DN"]), (OFF["DN"], NW)]


def wview(wb, name, idx, pat, **kw):
    bi, bs = idx
    off = OFF[name] + bi * bs
    for si, (s0, s1) in enumerate(SEGS):
        if s0 <= off < s1:
            return wb[si][off - s0:off - s0 + bs].rearrange(pat, **kw)
    raise ValueError(name)


def cast_jobs(src_all, wb, seg_ids, W=8192):
    jobs = []
    for si in seg_ids:
        s0, s1 = SEGS[si]
        F = (s1 - s0) // 128
        src = src_all[s0:s1].rearrange("(p f) -> p f", p=128)
        dst = wb[si].rearrange("(p f) -> p f", p=128)
        for f0 in range(0, F, W):
            jobs.append((src, dst, f0, min(W, F - f0)))
    return jobs


def st_cast(kb, jobs):
    W = 8192
    with ExitStack() as st:
        fb = [kb.sb(st, "cf%d" % i, [128, W], F32) for i in range(3)]
        bb = [kb.sb(st, "cb%d" % i, [128, W], BF16) for i in range(3)]
        for i, (src, dst, f0, w) in enumerate(jobs):
            s = i % 3
            kb.dma("sp", fb[s][:, :w], src[:, f0:f0 + w], writes=[fb[s]])
            if i % 2 == 0:
                kb.op("dve", lambda e, s=s, w=w: e.tensor_copy(out=bb[s][:, :w], in_=fb[s][:, :w]),
                      reads=[fb[s]], writes=[bb[s]])
            else:
                kb.op("act", lambda e, s=s, w=w: e.activation(out=bb[s][:, :w], in_=fb[s][:, :w], func=AF.Copy),
                      reads=[fb[s]], writes=[bb[s]])
            kb.dma("pool", dst[:, f0:f0 + w], bb[s][:, :w], reads=[bb[s]])
        kb.flush()


def st_inproj(kb, xin, wb, zT, cf32, bg_jobs=()):
    with ExitStack() as st:
        BW = 8192
        bg = {"next_load": 0, "next_cast": 0}
        if bg_jobs:
            fbg = [kb.sb(st, "fbg%d" % i, [128, BW], F32) for i in range(2)]
            bbg = [kb.sb(st, "bbg%d" % i, [128, BW], BF16) for i in range(2)]

        def bg_load():
            i = bg["next_load"]
            if i >= len(bg_jobs):
                return
            src, dst, f0, w = bg_jobs[i]
            kb.dma("pool", fbg[i % 2][:, :w], src[:, f0:f0 + w], writes=[fbg[i % 2]])
            bg["next_load"] += 1

        def bg_step():
            i = bg["next_cast"]
            if i >= len(bg_jobs):
                return
            bg_load()
            src, dst, f0, w = bg_jobs[i]
            s_ = i % 2
            eng = ("dve", "act")[i % 2]
            if eng == "act":
                kb.op("act", lambda e, s_=s_, w=w: e.activation(out=bbg[s_][:, :w], in_=fbg[s_][:, :w], func=AF.Copy),
                      reads=[fbg[s_]], writes=[bbg[s_]])
            else:
                kb.op(eng, lambda e, s_=s_, w=w: e.tensor_copy(out=bbg[s_][:, :w], in_=fbg[s_][:, :w]),
                      reads=[fbg[s_]], writes=[bbg[s_]])
            kb.dma("pool", dst[:, f0:f0 + w], bbg[s_][:, :w], reads=[bbg[s_]])
            bg["next_cast"] += 1
        if bg_jobs:
            bg_load()
        ident = kb.sb(st, "ident", [128, 128], F32)
        kb.dma("sp", ident[:], cf32[:, CF_ID:CF_ID + 128], writes=[ident])
        xT = kb.sb(st, "xT", [128, 16, 2048], BF16)
        xt = [kb.sb(st, "xt%d" % i, [128, 2048], F32) for i in range(2)]
        ptr = [kb.ps(st, "ptr%d" % i, [128, 4, 128], F32) for i in range(2)]
        pacc = [kb.ps(st, "pacc%d" % i, [128, 512], F32) for i in range(4)]
        wp = [kb.sb(st, "wp%d" % i, [128, 16, 128], BF16) for i in range(3)]
        ob = [kb.sb(st, "ob%d" % i, [128, 2048], BF16) for i in range(2)]
        ev = 0
        for half in range(2):
            for tt in range(16):
                tok0 = half * 2048 + tt * 128
                x_ = xt[tt % 2]
                kb.dma("sp", x_[:], xin[tok0:tok0 + 128, :], writes=[x_])
                for k4 in range(4):
                    pb = ptr[(tt * 4 + k4) % 2]
                    for j in range(4):
                        kc = k4 * 4 + j
                        kb.op("pe", lambda e, pb=pb, j=j, x_=x_, kc=kc: e.transpose(
                            out=pb[:, j, :], in_=x_[:, kc * 128:(kc + 1) * 128], identity=ident[:]),
                            reads=[x_, ident], writes=[pb], wadd=(j > 0))
                    oap = xT[:, k4 * 4:(k4 + 1) * 4, tt * 128:(tt + 1) * 128]
                    first = (tt == 0 and k4 == 0)
                    if ev % 2 == 0:
                        kb.op("act", lambda e, oap=oap, pb=pb: e.activation(out=oap, in_=pb[:], func=AF.Copy),
                              reads=[pb], writes=[xT], wadd=not first)
                    else:
                        kb.op("dve", lambda e, oap=oap, pb=pb: e.tensor_copy(out=oap, in_=pb[:]),
                              reads=[pb], writes=[xT], wadd=not first)
                    ev += 1
            for pn in range(109):
                if bg_jobs and pn % 2 == 0:
                    bg_step()
                M = 128 if pn < 108 else 48
                w = wp[pn % 3]
                if pn < 108:
                    src = wview(wb, "IN", (pn, 128 * 16 * 128), "(p k n) -> p k n", p=128, k=16)
                else:
                    src = wview(wb, "INL", (0, 128 * 16 * 48), "(p k n) -> p k n", p=128, k=16)
                kb.dma("sp", w[:, :, :M], src, writes=[w])
                o = ob[pn % 2]
                r0 = pn * 128
                if r0 >= ROW["mg"]:
                    func, scale = AF.Sigmoid, 1.0
                elif ROW["q"] <= r0 < ROW["kc"]:
                    func, scale = AF.Copy, 0.125
                else:
                    func, scale = AF.Copy, 1.0
                for tb in range(4):
                    pa = pacc[(pn * 4 + tb) % 4]
                    for kc in range(16):
                        kb.op("pe", lambda e, pa=pa, w=w, kc=kc, tb=tb, M=M: e.matmul(
                            pa[:M, :], lhsT=w[:, kc, :M], rhs=xT[:, kc, tb * 512:(tb + 1) * 512],
                            start=(kc == 0), stop=(kc == 15)),
                            reads=[w, xT], writes=[pa], wadd=(kc > 0))
                    oap = o[:M, tb * 512:(tb + 1) * 512]
                    if func == AF.Sigmoid or scale != 1.0 or ev % 2 == 0:
                        kb.op("act", lambda e, oap=oap, pa=pa, M=M, func=func, scale=scale: e.activation(
                            out=oap, in_=pa[:M, :], func=func, scale=scale),
                            reads=[pa], writes=[o], wadd=(tb > 0))
                    else:
                        kb.op("dve", lambda e, oap=oap, pa=pa, M=M: e.tensor_copy(out=oap, in_=pa[:M, :]),
                              reads=[pa], writes=[o], wadd=(tb > 0))
                    ev += 1
                kb.dma("pool", zT[r0:r0 + M, half * 2048:(half + 1) * 2048], o[:M, :], reads=[o])
        while bg_jobs and bg["next_cast"] < len(bg_jobs):
            bg_step()
        kb.flush()


def st_conv(kb, zT, yT, spl):
    with ExitStack() as st:
        cw = kb.sb(st, "cw", [128, 32], F32)
        kb.dma("sp", cw[:], spl[:, SP["CW"][0]:SP["CW"][0] + 32], writes=[cw])
        ain = [kb.sb(st, "ain%d" % i, [128, T], BF16) for i in range(2)]
        ab = [kb.sb(st, "ab%d" % i, [128, T], BF16) for i in range(2)]
        ac = [kb.sb(st, "ac%d" % i, [128, T], BF16) for i in range(2)]
        u = [kb.sb(st, "u%d" % i, [128, T + 2], F32) for i in range(2)]
        acc = [kb.sb(st, "acc%d" % i, [128, T], F32) for i in range(2)]
        yo = [kb.sb(st, "yo%d" % i, [128, T], BF16) for i in range(2)]
        for i in range(2):
            kb.op("dve", lambda e, i=i: e.memset(u[i][:, 0:2], 0.0), writes=[u[i]])
        for c in range(8):
            s = c % 2
            eng = "dve"
            kb.dma("sp", ain[s][:], zT[ROW["a_in"] + c * 128:ROW["a_in"] + (c + 1) * 128, :], writes=[ain[s]])
            kb.dma("sp", ab[s][:], zT[ROW["a_b"] + c * 128:ROW["a_b"] + (c + 1) * 128, :], writes=[ab[s]])
            kb.dma("sp", ac[s][:], zT[ROW["a_c"] + c * 128:ROW["a_c"] + (c + 1) * 128, :], writes=[ac[s]])
            us, accs, ys = u[s], acc[s], yo[s]
            kb.op("pool", lambda e, us=us, s=s: e.tensor_tensor(out=us[:, 2:T + 2], in0=ac[s][:], in1=ain[s][:], op=ALU.mult),
                  reads=[ac[s], ain[s]], writes=[us], wadd=True)
            kb.op(eng, lambda e, us=us, accs=accs, c=c: e.tensor_scalar(
                out=accs[:], in0=us[:, 0:T], scalar1=cw[:, c * 3:c * 3 + 1], scalar2=cw[:, 24 + c:25 + c],
                op0=ALU.mult, op1=ALU.add), reads=[us, cw], writes=[accs])
            for j in (1, 2):
                kb.op(eng, lambda e, us=us, accs=accs, c=c, j=j: e.scalar_tensor_tensor(
                    out=accs[:], in0=us[:, j:T + j], scalar=cw[:, c * 3 + j:c * 3 + j + 1], in1=accs[:],
                    op0=ALU.mult, op1=ALU.add), reads=[us, cw, accs], writes=[accs])
            kb.op("pool", lambda e, ys=ys, accs=accs, s=s: e.tensor_tensor(out=ys[:], in0=accs[:], in1=ab[s][:], op=ALU.mult),
                  reads=[accs, ab[s]], writes=[ys])
            kb.dma("pool" if s == 0 else "sp", yT[c * 128:(c + 1) * 128, :], ys[:], reads=[ys])
        kb.flush()


def st_lru(kb, zT, yT, spl, wb):
    with ExitStack() as st:
        o0 = SP["LW"][0]
        n0 = SP["LAM"][0] + 8 - o0
        sm = kb.sb(st, "sm", [128, n0], F32)
        kb.dma("sp", sm[:], spl[:, o0:o0 + n0], writes=[sm])
        LW, LB, BA, BX, LAM = 0, 32, 40, 48, 56
        lw = kb.sb(st, "lruw", [128, 2, 8, 128], BF16)
        kb.dma("sp", lw[:], wview(wb, "LRU", (0, 128 * 2048), "(p a h j) -> p a h j", p=128, a=2, h=8), writes=[lw])
        cv = kb.sb(st, "cv", [128, 3, 8], F32)
        kb.op("act", lambda e: e.activation(out=cv[:, 0, :], in_=sm[:, LAM:LAM + 8], func=AF.Exp, scale=-1.0),
              reads=[sm], writes=[cv])
        kb.op("act", lambda e: e.activation(out=cv[:, 0, :], in_=cv[:, 0, :], func=AF.Ln, bias=1.0),
              reads=[cv], writes=[cv])
        kb.op("dve", lambda e: e.tensor_scalar(out=cv[:, 1, :], in0=cv[:, 0, :], scalar1=-8.0, scalar2=None, op0=ALU.mult),
              reads=[cv], writes=[cv])
        kb.op("dve", lambda e: e.tensor_scalar(out=cv[:, 2, :], in0=cv[:, 0, :], scalar1=-16.0, scalar2=None, op0=ALU.mult),
              reads=[cv], writes=[cv])
        rin = kb.sb(st, "rin", [128, T], BF16)
        rg = kb.sb(st, "rg", [128, T], BF16)
        up = kb.sb(st, "up", [128, T + 3], F32)
        xc = kb.sb(st, "xc", [128, T], F32)
        xcb = kb.sb(st, "xcb", [128, T], BF16)
        r = kb.sb(st, "r", [128, T], F32)
        ii = kb.sb(st, "ii", [128, T], F32)
        a = kb.sb(st, "a", [128, T], F32)
        a2 = kb.sb(st, "a2", [128, T], F32)
        yo = kb.sb(st, "yo", [128, T], BF16)
        pr = [kb.ps(st, "pr%d" % i, [128, 512], F32) for i in range(4)]
        kb.op("dve", lambda e: e.memset(up[:, 0:3], 0.0), writes=[up])
        for h in range(8):
            kb.dma("sp", rin[:], zT[ROW["r_in"] + h * 128:ROW["r_in"] + (h + 1) * 128, :], writes=[rin])
            kb.dma("sp", rg[:], zT[ROW["r_gate"] + h * 128:ROW["r_gate"] + (h + 1) * 128, :], writes=[rg])
            kb.op("pool", lambda e: e.tensor_copy(out=up[:, 3:T + 3], in_=rin[:]), reads=[rin], writes=[up], wadd=True)
            kb.op("dve", lambda e, h=h: e.tensor_scalar(
                out=xc[:], in0=up[:, 0:T], scalar1=sm[:, LW + h * 4:LW + h * 4 + 1], scalar2=sm[:, LB + h:LB + h + 1],
                op0=ALU.mult, op1=ALU.add), reads=[up, sm], writes=[xc])
            for j in (1, 2, 3):
                kb.op("dve", lambda e, h=h, j=j: e.scalar_tensor_tensor(
                    out=xc[:], in0=up[:, j:T + j], scalar=sm[:, LW + h * 4 + j:LW + h * 4 + j + 1], in1=xc[:],
                    op0=ALU.mult, op1=ALU.add), reads=[up, sm, xc], writes=[xc])
            kb.op("act", lambda e: e.activation(out=xcb[:], in_=xc[:], func=AF.Copy), reads=[xc], writes=[xcb])
            for tb in range(8):
                pa, px = pr[(tb * 2) % 4], pr[(tb * 2 + 1) % 4]
                sl = slice(tb * 512, (tb + 1) * 512)
                kb.op("pe", lambda e, pa=pa, h=h, sl=sl: e.matmul(pa[:], lhsT=lw[:, 0, h, :], rhs=xcb[:, sl], start=True, stop=True),
                      reads=[lw, xcb], writes=[pa])
                kb.op("pe", lambda e, px=px, h=h, sl=sl: e.matmul(px[:], lhsT=lw[:, 1, h, :], rhs=xcb[:, sl], start=True, stop=True),
                      reads=[lw, xcb], writes=[px])
                kb.op("act", lambda e, pa=pa, h=h, sl=sl: e.activation(out=r[:, sl], in_=pa[:], func=AF.Sigmoid, bias=sm[:, BA + h:BA + h + 1]),
                      reads=[pa, sm], writes=[r], wadd=(tb > 0))
                kb.op("act", lambda e, px=px, h=h, sl=sl: e.activation(out=ii[:, sl], in_=px[:], func=AF.Sigmoid, bias=sm[:, BX + h:BX + h + 1]),
                      reads=[px, sm], writes=[ii], wadd=(tb > 0))
            kb.op("act", lambda e, h=h: e.activation(out=a[:], in_=r[:], func=AF.Exp, scale=cv[:, 1, h:h + 1]),
                  reads=[r, cv], writes=[a])
            kb.op("act", lambda e, h=h: e.activation(out=a2[:], in_=r[:], func=AF.Exp, scale=cv[:, 2, h:h + 1]),
                  reads=[r, cv], writes=[a2])
            kb.op("dve", lambda e: e.tensor_scalar(out=a2[:], in0=a2[:], scalar1=1.0, scalar2=-1.0, op0=ALU.min, op1=ALU.mult),
                  reads=[a2], writes=[a2])
            kb.op("act", lambda e: e.activation(out=a2[:], in_=a2[:], func=AF.Sqrt, bias=1.0, scale=1.0), reads=[a2], writes=[a2])
            kb.op("pool", lambda e: e.tensor_tensor(out=ii[:], in0=ii[:], in1=xc[:], op=ALU.mult), reads=[ii, xc], writes=[ii])
            kb.op("dve", lambda e: e.tensor_tensor(out=a2[:], in0=a2[:], in1=ii[:], op=ALU.mult), reads=[a2, ii], writes=[a2])
            kb.op("dve", lambda e: e.tensor_tensor_scan(out=r[:], data0=a[:], data1=a2[:], initial=0.0, op0=ALU.mult, op1=ALU.add),
                  reads=[a, a2], writes=[r])
            kb.op("act", lambda e: e.activation(out=xc[:], in_=rg[:], func=AF.Gelu_apprx_tanh), reads=[rg], writes=[xc])
            kb.op("dve", lambda e: e.tensor_tensor(out=yo[:], in0=r[:], in1=xc[:], op=ALU.mult), reads=[r, xc], writes=[yo])
            kb.dma("pool", yT[1024 + h * 128:1024 + (h + 1) * 128, :], yo[:], reads=[yo])
        kb.flush()


def st_nsa(kb, zT, yT, wb, spl, cbf, cf32, kaug, kaugc, qaug):
    with ExitStack() as st:
        cb = kb.sb(st, "cbt", [128, NCB], BF16)
        kb.dma("sp", cb[:], cbf, writes=[cb])
        cf = kb.sb(st, "cft", [128, NCF], F32)
        kb.dma("sp", cf[:], cf32, writes=[cf])
        b1 = kb.sb(st, "b1", [128, 2], F32)
        kb.dma("sp", b1[:], spl[:, SP["B1"][0]:SP["B1"][0] + 2], writes=[b1])
        identb = cb[:, CB_ID:CB_ID + 128]
        caus4 = cb[:, CB_CAUS:CB_CAUS + 128].unsqueeze(1).broadcast_to([128, 4, 128])
        tail4 = cb[:, CB_TAIL:CB_TAIL + 128].unsqueeze(1).broadcast_to([128, 4, 128])
        ksT = kb.sb(st, "ksT", [69, 4, T], BF16)
        kwT = kb.sb(st, "kwT", [69, 4, T], BF16)
        kcT = kb.sb(st, "kcT", [69, 4, 256], BF16)
        Vs = kb.sb(st, "Vs", [128, 32, 4, 65], BF16)
        Vw = kb.sb(st, "Vw", [128, 32, 4, 65], BF16)
        Vc = kb.sb(st, "Vc", [128, 2, 4, 65], BF16)
        gT = kb.sb(st, "gT", [48, T], BF16)
        zr = kb.sb(st, "zr", [1, 512], BF16)
        st2 = ExitStack()
        big = kb.sb(st2, "big", [128, 4, T], BF16)
        w1 = kb.sb(st2, "w1", [64, 2, 32, 128], BF16)
        w2 = kb.sb(st2, "w2", [128, 2, 64], BF16)
        posT = kb.sb(st2, "posT", [64, 2, 32], BF16)
        hid = kb.sb(st2, "hid", [128, 256], BF16)
        bt = kb.sb(st2, "bt", [128, 2], F32)
        S = [kb.ps(st, "S%d" % i, [128, 4, 128], F32) for i in range(2)]
        OA = [kb.ps(st, "OA%d" % i, [128, 4, 128], F32) for i in range(2)]
        IB = kb.ps(st, "IB", [128, 4, 128], F32)
        MB = kb.ps(st, "MB", [128, 8, 128], BF16)
        MF = kb.ps(st, "MF", [128, 512], F32)

        kb.op("dve", lambda e: e.memset(zr[:], 0.0), writes=[zr])
        kb.op("dve", lambda e: e.memset(hid[:], 0.0), writes=[hid])
        for V in (Vs, Vw, Vc):
            kb.op("pool", lambda e, V=V: e.memset(V[:, :, :, 64:65], 1.0), writes=[V])
        kb.dma("sp", ksT[0:64, :, :], zT[ROW["ks"]:ROW["ks"] + 256, :].rearrange("(g d) t -> d g t", d=64), writes=[ksT])
        kb.dma("sp", ksT[64:69, :, :], kaug, writes=[ksT], wadd=True)
        kb.dma("sp", kwT[0:64, :, :], zT[ROW["kw"]:ROW["kw"] + 256, :].rearrange("(g d) t -> d g t", d=64), writes=[kwT])
        kb.dma("sp", kwT[64:69, :, :], kaug, writes=[kwT], wadd=True)
        kb.dma("sp", kcT[64:69, :, :], kaugc, writes=[kcT])
        kb.dma("sp", gT[:], zT[ROW["ng"]:ROW["ng"] + 48, :], writes=[gT])
        kb.dma("sp", w1[:], wview(wb, "W1", (0, 64 * 8192), "(p s j h) -> p s j h", p=64, s=2, j=32), writes=[w1])
        kb.dma("sp", w2[:], wview(wb, "W2", (0, 128 * 128), "(p s d) -> p s d", p=128, s=2), writes=[w2])
        kb.dma("sp", posT[:], wview(wb, "POS", (0, 64 * 64), "(p s j) -> p s j", p=64, s=2), writes=[posT])
        for (V, rname) in ((Vs, "vs"), (Vw, "vw")):
            kb.dma("sp", big[:, 0:2, :], zT[ROW[rname]:ROW[rname] + 256, :].rearrange("(a p) t -> p a t", p=128), writes=[big])
            for k4 in range(8):
                for kk in range(4):
                    kt = k4 * 4 + kk
                    for a_ in range(2):
                        kb.op("pe", lambda e, kk=kk, a_=a_, kt=kt: e.transpose(
                            out=MB[:, kk * 2 + a_, :], in_=big[:, a_, kt * 128:(kt + 1) * 128], identity=identb),
                            reads=[big, cb], writes=[MB], wadd=not (kk == 0 and a_ == 0))
                kb.op("dve" if k4 % 2 == 0 else "act",
                      (lambda e, V=V, k4=k4: e.tensor_copy(
                          out=V[:, k4 * 4:(k4 + 1) * 4, :, 0:64],
                          in_=MB[:].rearrange("p (k a) (g d) -> p k (a g) d", k=4, g=2))) if k4 % 2 == 0 else
                      (lambda e, V=V, k4=k4: e.activation(
                          out=V[:, k4 * 4:(k4 + 1) * 4, :, 0:64],
                          in_=MB[:].rearrange("p (k a) (g d) -> p k (a g) d", k=4, g=2), func=AF.Copy)),
                      reads=[MB], writes=[V], wadd=True)
        for s in range(2):
            for j in range(32):
                kb.op("pe", lambda e, s=s, j=j: e.matmul(MF[:, s:s + 1], lhsT=w1[:, s, j, :], rhs=posT[:, s, j:j + 1],
                                                         start=(j == 0), stop=(j == 31)),
                      reads=[w1, posT], writes=[MF], wadd=not (s == 0 and j == 0))
        kb.op("dve", lambda e: e.tensor_tensor(out=bt[:], in0=MF[:, 0:2], in1=b1[:], op=ALU.add), reads=[MF, b1], writes=[bt])
        for s, rname in ((0, "kc"), (1, "vc")):
            kb.dma("sp", big[0:64, :, :], zT[ROW[rname]:ROW[rname] + 256, :].rearrange("(g d) t -> d g t", d=64), writes=[big])
            for g in range(4):
                for j in range(32):
                    kb.op("pe", lambda e, s=s, j=j, g=g: e.matmul(
                        MF[:, 0:255], lhsT=w1[:, s, j, :], rhs=big[0:64, g, j:j + 16 * 254 + 1:16],
                        start=(j == 0), stop=(j == 31)), reads=[w1, big], writes=[MF], wadd=(j > 0))
                kb.op("act", lambda e, s=s: e.activation(out=hid[:, 0:255], in_=MF[:, 0:255], func=AF.Gelu_apprx_tanh,
                                                         bias=bt[:, s:s + 1]), reads=[MF, bt], writes=[hid])
                if s == 0:
                    kb.op("pe", lambda e: e.matmul(MF[0:64, 256:512], lhsT=w2[:, 0, :], rhs=hid[:], start=True, stop=True),
                          reads=[w2, hid], writes=[MF])
                    kb.op("dve", lambda e, g=g: e.tensor_copy(out=kcT[0:64, g, :], in_=MF[0:64, 256:512]),
                          reads=[MF], writes=[kcT], wadd=True)
                else:
                    for nt in range(2):
                        kb.op("pe", lambda e, nt=nt: e.matmul(MF[:, 256 + nt * 64:256 + (nt + 1) * 64],
                                                              lhsT=hid[:, nt * 128:(nt + 1) * 128], rhs=w2[:, 1, :],
                                                              start=True, stop=True), reads=[w2, hid], writes=[MF], wadd=(nt > 0))
                    kb.op("dve", lambda e, g=g: e.tensor_copy(
                        out=Vc[:, :, g, 0:64], in_=MF[:, 256:384].rearrange("p (n d) -> p n d", n=2)),
                        reads=[MF], writes=[Vc], wadd=True)

        kb.flush()
        st2.close()
        qT = [kb.sb(st, "qT%d" % i, [69, 16, 128], BF16) for i in range(2)]
        gt = [kb.sb(st, "gt%d" % i, [128, 16, 3], F32) for i in range(2)]
        yc = [kb.sb(st, "yc%d" % i, [128, 1024], F32) for i in range(2)]
        ycb = kb.sb(st, "ycb", [128, 1024], BF16)
        ycT = [kb.sb(st, "ycT%d" % i, [128, 8, 128], BF16) for i in range(2)]
        pT = [kb.sb(st, "pT%d" % i, [128, 4, 128], BF16) for i in range(3)]
        mbT = [kb.sb(st, "mbT%d" % i, [64, 128], BF16) for i in range(2)]
        rd = kb.sb(st, "rd", [128, 4], F32)
        coef = kb.sb(st, "coef", [128, 4], F32)
        imp = kb.sb(st, "imp", [128, 64], F32)
        impm = kb.sb(st, "impm", [128, 64], F32)
        wk = kb.sb(st, "wk", [128, 64], F32)
        m8 = kb.sb(st, "m8", [128, 8], F32)
        m8b = kb.sb(st, "m8b", [128, 8], F32)
        mb = kb.sb(st, "mb", [128, 64], BF16)
        cnt = {"S": 0, "OA": 0, "p": 0, "mb": 0}

        pend = []

        def drain():
            for f in pend:
                f()
            pend.clear()

        def zero_init(P, ncol):
            kb.op("pe", lambda e, P=P, ncol=ncol: e.matmul(
                P[:, :, 0:ncol], lhsT=zr[0:1, 0:128], rhs=zr[0:1, 0:4 * ncol].rearrange("p (a b) -> p a b", a=4),
                start=True, stop=False, skip_group_check=True), reads=[zr], writes=[P])

        def unit(q, g, lhs_main, extra, V_ap, P, IBp=None, CM_ap=None, last=False):
            Sb = S[cnt["S"] % 2]
            cnt["S"] += 1
            n_ex = len(extra)
            kb.op("pe", lambda e, Sb=Sb, lhs_main=lhs_main, q=q, g=g, n_ex=n_ex: e.matmul(
                Sb[:], lhsT=lhs_main, rhs=q[:, 4 * g:4 * g + 4, :], start=True, stop=(n_ex == 0)),
                reads=[q, ksT, kwT, kcT], writes=[Sb])
            for i, (l_, r_, rb) in enumerate(extra):
                kb.op("pe", lambda e, Sb=Sb, l_=l_, r_=r_, i=i, n_ex=n_ex: e.matmul(
                    Sb[:], lhsT=l_, rhs=r_, start=False, stop=(i == n_ex - 1)),
                    reads=[cb] + rb, writes=[Sb], wadd=True)
            p_ = pT[cnt["p"] % 3]
            cnt["p"] += 1
            kb.op("act", lambda e, p_=p_, Sb=Sb: e.activation(out=p_[:], in_=Sb[:], func=AF.Exp), reads=[Sb], writes=[p_])
            drain()

            def pv(p_=p_, P=P, V_ap=V_ap, IBp=IBp, CM_ap=CM_ap, last=last):
                for h in range(4):
                    kb.op("pe", lambda e, P=P, h=h, p_=p_, V_ap=V_ap: e.matmul(
                        P[:, h, 0:65], lhsT=p_[:, h, :], rhs=V_ap, start=False, stop=(last and h == 3 and IBp is None),
                        skip_group_check=True), reads=[p_, Vs, Vw, Vc], writes=[P], wadd=True)
                    if IBp is not None:
                        kb.op("pe", lambda e, IBp=IBp, h=h, p_=p_, CM_ap=CM_ap: e.matmul(
                            IBp[:, h, 0:64], lhsT=p_[:, h, :], rhs=CM_ap, start=False, stop=(last and h == 3),
                            skip_group_check=True), reads=[p_, cb], writes=[IBp], wadd=True)
            pend.append(pv)

        def finalize(P, g, br, gt_, yc_):
            kb.op("dve", lambda e, P=P: e.tensor_scalar(out=rd[:], in0=P[:, :, 64], scalar1=1e-30, scalar2=None, op0=ALU.max),
                  reads=[P], writes=[rd])
            kb.op("dve", lambda e: e.reciprocal(out=rd[:], in_=rd[:]), reads=[rd], writes=[rd])
            kb.op("dve", lambda e, g=g, br=br, gt_=gt_: e.tensor_tensor(out=coef[:], in0=rd[:], in1=gt_[:, 4 * g:4 * g + 4, br], op=ALU.mult),
                  reads=[rd, gt_], writes=[coef])
            for h in range(4):
                c0 = (4 * g + h) * 64
                if br == 0:
                    kb.op("dve", lambda e, P=P, h=h, c0=c0, yc_=yc_: e.tensor_scalar(
                        out=yc_[:, c0:c0 + 64], in0=P[:, h, 0:64], scalar1=coef[:, h:h + 1], scalar2=None, op0=ALU.mult),
                        reads=[P, coef], writes=[yc_], wadd=True)
                else:
                    kb.op("dve", lambda e, P=P, h=h, c0=c0, yc_=yc_: e.scalar_tensor_tensor(
                        out=yc_[:, c0:c0 + 64], in0=P[:, h, 0:64], scalar=coef[:, h:h + 1], in1=yc_[:, c0:c0 + 64],
                        op0=ALU.mult, op1=ALU.add), reads=[P, coef, yc_], writes=[yc_], wadd=True)

        for a in range(32):
            q = qT[a % 2]
            gt_ = gt[a % 2]
            yc_ = yc[a % 2]
            tsl = slice(a * 128, (a + 1) * 128)
            kb.dma("sp", q[0:64, :, :], zT[ROW["q"]:ROW["q"] + 1024, tsl].rearrange("(h d) t -> d h t", d=64), writes=[q])
            kb.dma("sp", q[64:69, :, :], qaug[:, :, tsl], writes=[q], wadd=True)
            kb.op("pe", lambda e, tsl=tsl: e.transpose(out=MB[:, 0, 0:48], in_=gT[:, tsl], identity=identb[0:48, 0:48]),
                  reads=[gT, cb], writes=[MB])
            kb.op("dve", lambda e, gt_=gt_: e.tensor_copy(out=gt_[:].rearrange("p h b -> p (h b)"), in_=MB[:, 0, 0:48]),
                  reads=[MB], writes=[gt_])
            for g in range(4):
                P = OA[cnt["OA"] % 2]
                cnt["OA"] += 1
                zero_init(P, 65)
                zero_init(IB, 64)
                ntc = 1 if a < 16 else 2
                for nt in range(ntc):
                    cm4 = cb[:, CB_MASK + (a * 2 + nt) * 128:CB_MASK + (a * 2 + nt + 1) * 128].unsqueeze(1).broadcast_to([128, 4, 128])
                    unit(q, g, kcT[:, g, nt * 128:(nt + 1) * 128], [(identb, cm4, [])], Vc[:, nt, g, :], P,
                         IBp=IB, CM_ap=cb[:, CB_CM + nt * 64:CB_CM + (nt + 1) * 64], last=(nt == ntc - 1))
                drain()
                finalize(P, g, 0, gt_, yc_)
                for h in range(4):
                    if h == 0:
                        kb.op("dve", lambda e: e.tensor_scalar(out=imp[:], in0=IB[:, 0, 0:64], scalar1=rd[:, 0:1], scalar2=None, op0=ALU.mult),
                              reads=[IB, rd], writes=[imp])
                    else:
                        kb.op("dve", lambda e, h=h: e.scalar_tensor_tensor(out=imp[:], in0=IB[:, h, 0:64], scalar=rd[:, h:h + 1], in1=imp[:],
                                                                         op0=ALU.mult, op1=ALU.add), reads=[IB, rd, imp], writes=[imp])
                j0 = 63 - 2 * a
                kb.op("dve", lambda e, j0=j0: e.tensor_tensor(out=impm[:], in0=imp[:], in1=cf[:, CF_WC + j0:CF_WC + j0 + 64], op=ALU.mult),
                      reads=[imp, cf], writes=[impm])
                kb.op("dve", lambda e, j0=j0: e.tensor_tensor(out=impm[:], in0=impm[:], in1=cf[:, CF_WF + j0:CF_WF + j0 + 64], op=ALU.add),
                      reads=[impm, cf], writes=[impm])
                kb.op("dve", lambda e: e.memset(impm[:, 0:1], 1e9), reads=[impm], writes=[impm])
                kb.op("dve", lambda e: e.max(out=m8[:], in_=impm[:]), reads=[impm], writes=[m8])
                kb.op("dve", lambda e: e.match_replace(out=wk[:], in_to_replace=m8[:], in_values=impm[:], imm_value=-3e9),
                      reads=[impm, m8], writes=[wk])
                kb.op("dve", lambda e: e.max(out=m8b[:], in_=wk[:]), reads=[wk], writes=[m8b])
                kb.op("dve", lambda e: e.tensor_scalar(out=wk[:], in0=impm[:], scalar1=m8b[:, 7:8], scalar2=None, op0=ALU.is_ge),
                      reads=[impm, m8b, wk], writes=[wk])
                kb.op("dve", lambda e: e.tensor_scalar(out=mb[:], in0=wk[:], scalar1=-NEGB, scalar2=NEGB, op0=ALU.mult, op1=ALU.add),
                      reads=[wk], writes=[mb])
                mbT_ = mbT[cnt["mb"] % 2]
                cnt["mb"] += 1
                kb.op("pe", lambda e: e.transpose(out=MB[0:64, 1, :], in_=mb[:], identity=identb), reads=[mb, cb], writes=[MB])
                kb.op("dve", lambda e, mbT_=mbT_: e.tensor_copy(out=mbT_[:], in_=MB[0:64, 1, :]), reads=[MB], writes=[mbT_])
                P = OA[cnt["OA"] % 2]
                cnt["OA"] += 1
                zero_init(P, 65)
                b0 = max(0, a - 4)
                for b in range(b0, a + 1):
                    ex = []
                    if b == a:
                        ex.append((identb, caus4, []))
                    if b == a - 4:
                        ex.append((identb, tail4, []))
                    unit(q, g, kwT[:, g, b * 128:(b + 1) * 128], ex, Vw[:, b, g, :], P, last=(b == a))
                pend.append(lambda P=P, g=g, gt_=gt_, yc_=yc_: finalize(P, g, 2, gt_, yc_))
                P = OA[cnt["OA"] % 2]
                cnt["OA"] += 1
                zero_init(P, 65)
                mb4 = mbT_[:].unsqueeze(1).broadcast_to([64, 4, 128])
                for b in range(0, a + 1):
                    ex = [(cb[0:64, CB_E + b * 128:CB_E + (b + 1) * 128], mb4, [mbT_])]
                    if b == a:
                        ex.append((identb, caus4, []))
                    unit(q, g, ksT[:, g, b * 128:(b + 1) * 128], ex, Vs[:, b, g, :], P, last=(b == a))
                pend.append(lambda P=P, g=g, gt_=gt_, yc_=yc_: finalize(P, g, 1, gt_, yc_))
            drain()
            kb.op("act", lambda e, yc_=yc_: e.activation(out=ycb[:], in_=yc_[:], func=AF.Copy), reads=[yc_], writes=[ycb])
            for c in range(8):
                kb.op("pe", lambda e, c=c: e.transpose(out=MB[:, c, :], in_=ycb[:, c * 128:(c + 1) * 128], identity=identb),
                      reads=[ycb, cb], writes=[MB], wadd=(c > 0))
            ycT_ = ycT[a % 2]
            kb.op("dve", lambda e, ycT_=ycT_: e.tensor_copy(out=ycT_[:], in_=MB[:]), reads=[MB], writes=[ycT_])
            kb.dma("pool", yT[2048:3072, tsl].rearrange("(c p) t -> p c t", p=128), ycT_[:], reads=[ycT_])
        kb.flush()


def ln_tile(kb, r, rsl, lnp, outb, osl, tmp):
    bs, mv, rs = tmp
    lng, lnb = lnp[:, 0, :], lnp[:, 1, :]
    for c in range(4):
        kb.op("dve", lambda e, c=c: e.bn_stats(out=bs[:, c, :], in_=r[rsl][:, c * 512:(c + 1) * 512]),
              reads=[r], writes=[bs], wadd=(c > 0))
    kb.op("dve", lambda e: e.bn_aggr(out=mv[:], in_=bs[:]), reads=[bs], writes=[mv])
    import os
    LNCUT = int(os.environ.get("LNCUT", "9"))
    if LNCUT < 2:
        kb.op("dve", lambda e: e.tensor_copy(out=outb[osl], in_=r[rsl]), reads=[r], writes=[outb])
        return
    kb.op("dve", lambda e: e.tensor_scalar(out=rs[:], in0=mv[:, 1:2], scalar1=LN_EPS, scalar2=None, op0=ALU.add),
          reads=[mv], writes=[rs])
    kb.op("act", lambda e: e.activation(out=rs[:], in_=rs[:], func=AF.Sqrt), reads=[rs], writes=[rs])
    kb.op("dve", lambda e: e.reciprocal(out=rs[:], in_=rs[:]), reads=[rs], writes=[rs])
    if LNCUT < 3:
        kb.op("dve", lambda e: e.tensor_copy(out=outb[osl], in_=r[rsl]), reads=[r], writes=[outb])
        return
    kb.op("dve", lambda e: e.tensor_scalar(out=r[rsl], in0=r[rsl], scalar1=mv[:, 0:1], scalar2=rs[:, 0:1],
                                           op0=ALU.subtract, op1=ALU.mult), reads=[r, mv, rs], writes=[r], wadd=True)
    if LNCUT < 4:
        kb.op("dve", lambda e: e.tensor_copy(out=outb[osl], in_=r[rsl]), reads=[r], writes=[outb])
        return
    kb.op("dve", lambda e: e.tensor_tensor(out=r[rsl], in0=r[rsl], in1=lng, op=ALU.mult), reads=[r, lnp], writes=[r], wadd=True)
    kb.op("dve", lambda e: e.tensor_tensor(out=outb[osl], in0=r[rsl], in1=lnb, op=ALU.add), reads=[r, lnp], writes=[outb])


def st_mix(kb, xin, zT, yT, wb, spl, cf32, xcur1, x1T, gates, cbf, tokid, oob, lst, ridx_d, gsel_d, x1b_d):
    with ExitStack() as st:
        dl = Buf("lst", None)
        kb.dma("sp", lst.rearrange("(p f) o -> p (f o)", p=128), oob.rearrange("(p f) o -> p (f o)", p=128), writes=[dl])
        lo = kb.sb(st, "lo", [128, 256], BF16)
        kb.dma("sp", lo[:], cbf[:, CB_L:CB_L + 256], writes=[lo])
        rowb = kb.sb(st, "rowb", [128, 16], F32)
        kb.dma("sp", rowb[:], cf32[:, CF_RB:CF_RB + 16], writes=[rowb])
        tki = kb.sb(st, "tki", [128, 32], I32)
        kb.dma("sp", tki[:], tokid, writes=[tki])
        chb = kb.sb(st, "chb", [128, 16], BF16)
        r12i = [kb.sb(st, "r12i%d" % i, [128, 2], I32) for i in range(2)]
        g12 = [kb.sb(st, "g12_%d" % i, [128, 2], F32) for i in range(2)]
        x1b = [kb.sb(st, "x1b%d" % i, [128, 2048], BF16) for i in range(2)]
        tk1 = [kb.sb(st, "tk1_%d" % i, [128, 1], I32) for i in range(2)]
        r1c = [kb.sb(st, "r1c%d" % i, [128, 1], I32) for i in range(4)]
        ident = kb.sb(st, "ident", [128, 128], F32)
        kb.dma("sp", ident[:], cf32[:, CF_ID:CF_ID + 128], writes=[ident])
        lnp = kb.sb(st, "lnp", [128, 2, 2048], F32)
        kb.dma("sp", lnp[:, 0, :], spl[:, SP["LNG"][0]:SP["LNG"][0] + 2048], writes=[lnp])
        kb.dma("sp", lnp[:, 1, :], spl[:, SP["LNB"][0]:SP["LNB"][0] + 2048], writes=[lnp], wadd=True)
        wr = kb.sb(st, "wr", [128, 16, 16], F32)
        kb.dma("sp", wr[:], spl[:, SP["WR"][0]:SP["WR"][0] + 256].rearrange("p (k e) -> p k e", k=16), writes=[wr])
        brt = kb.sb(st, "brt", [128, 16], F32)
        kb.dma("sp", brt[:], spl[:, SP["BR"][0]:SP["BR"][0] + 16], writes=[brt])
        yTb = kb.sb(st, "yTb", [128, 24, 512], BF16)
        xt = kb.sb(st, "xt", [128, 4, 2048], F32)
        mT = kb.sb(st, "mT", [128, 16, 512], BF16)
        gl = [kb.sb(st, "gl%d" % i, [128, 3, 512], BF16) for i in range(2)]
        wbr = [kb.sb(st, "wbr%d" % i, [128, 3, 8, 128], BF16) for i in range(2)]
        tm = [kb.sb(st, "tm%d" % i, [128, 512], F32) for i in range(3)]
        wo = [kb.sb(st, "wo%d" % i, [128, 16, 512], BF16) for i in range(2)]
        x1 = [kb.sb(st, "x1_%d" % i, [128, 2048], F32) for i in range(2)]
        xf = kb.sb(st, "xf", [128, 16, 128], F32)
        x1Tb = kb.sb(st, "x1Tb", [128, 16, 512], BF16)
        bs = kb.sb(st, "bs", [128, 4, 6], F32)
        mv = kb.sb(st, "mv", [128, 2], F32)
        rs = kb.sb(st, "rs", [128, 1], F32)
        R = {n: kb.sb(st, "rt_" + n, sh, F32) for n, sh in
             [("aff", [128, 16]), ("sel", [128, 16]), ("m1", [128, 4]), ("eq", [128, 16]), ("sel2", [128, 16]),
              ("m2", [128, 4]), ("gm", [128, 1]), ("t1", [128, 1]), ("t2", [128, 1]), ("is1", [128, 16]),
              ("den", [128, 1]), ("is2", [128, 16]), ("ch", [128, 16]), ("pos", [128, 16]), ("ovf", [128, 16]),
              ("tmp", [128, 16]), ("r12", [128, 2]), ("g12", [128, 2]), ("cntb", [128, 16])]}
        gate = [kb.sb(st, "gate%d" % i, [128, 16], F32) for i in range(2)]
        pacc = [kb.ps(st, "pbr%d" % i, [128, 512], F32) for i in range(3)]
        pout = [kb.ps(st, "pout%d" % i, [128, 512], F32) for i in range(2)]
        ptr = [kb.ps(st, "ptr%d" % i, [128, 4, 128], F32) for i in range(2)]
        prt = kb.ps(st, "prt", [128, 512], F32)
        mgv = zT[ROW["mg"]:ROW["mg"] + 6144, :].rearrange("(i n p) t -> p i n t", i=3, p=128)
        trc = 0
        for tbk in range(8):
            tsl = slice(tbk * 512, (tbk + 1) * 512)
            kb.dma("sp", yTb[:], yT[0:3072, tsl].rearrange("(k p) t -> p k t", p=128), writes=[yTb])
            kb.dma("sp", xt[:], xin[tsl, :].rearrange("(a p) d -> p a d", p=128), writes=[xt])
            for nch in range(16):
                g_ = gl[nch % 2]
                w_ = wbr[nch % 2]
                kb.dma("sp", g_[:], mgv[:, :, nch, tsl], writes=[g_])
                kb.dma("sp", w_[:], wview(wb, "BR", (nch, 128 * 3 * 8 * 128), "(p i k n) -> p i k n", p=128, i=3, k=8), writes=[w_])
                for i in range(3):
                    for kc in range(8):
                        kb.op("pe", lambda e, i=i, kc=kc, w_=w_: e.matmul(
                            pacc[i][:], lhsT=w_[:, i, kc, :], rhs=yTb[:, i * 8 + kc, :], start=(kc == 0), stop=(kc == 7)),
                            reads=[w_, yTb], writes=[pacc[i]], wadd=(kc > 0))
                for i in range(3):
                    kb.op("dve", lambda e, i=i, g_=g_: e.tensor_tensor(out=tm[i][:], in0=pacc[i][:], in1=g_[:, i, :], op=ALU.mult),
                          reads=[pacc[i], g_], writes=[tm[i]])
                kb.op("pool", lambda e: e.tensor_tensor(out=tm[0][:], in0=tm[0][:], in1=tm[1][:], op=ALU.add),
                      reads=[tm[0], tm[1]], writes=[tm[0]])
                kb.op("pool", lambda e, nch=nch: e.tensor_tensor(out=mT[:, nch, :], in0=tm[0][:], in1=tm[2][:], op=ALU.add),
                      reads=[tm[0], tm[2]], writes=[mT], wadd=(nch > 0))
            for n4 in range(4):
                w_ = wo[n4 % 2]
                kb.dma("sp", w_[:], wview(wb, "OUT", (n4, 128 * 16 * 512), "(p k n) -> p k n", p=128, k=16), writes=[w_])
                for tt in range(4):
                    po = pout[(n4 * 4 + tt) % 2]
                    for kc in range(16):
                        kb.op("pe", lambda e, po=po, kc=kc, tt=tt, w_=w_: e.matmul(
                            po[:], lhsT=mT[:, kc, tt * 128:(tt + 1) * 128], rhs=w_[:, kc, :], start=(kc == 0), stop=(kc == 15)),
                            reads=[mT, w_], writes=[po], wadd=(kc > 0))
                    kb.op("dve", lambda e, po=po, tt=tt, n4=n4: e.scalar_tensor_tensor(
                        out=xt[:, tt, n4 * 512:(n4 + 1) * 512], in0=xt[:, tt, n4 * 512:(n4 + 1) * 512], scalar=ALPHA, in1=po[:],
                        op0=ALU.mult, op1=ALU.add), reads=[xt, po], writes=[xt], wadd=True)

            def phase3(tt):
                nonlocal trc
                x1_ = x1[tt % 2]
                tok0 = tbk * 512 + tt * 128
                ln_tile(kb, xt, (slice(None), tt, slice(None)), lnp, x1_, (slice(None), slice(None)), (bs, mv, rs))
                kb.dma("pool", xcur1[tok0:tok0 + 128, :], x1_[:], reads=[x1_])
                for k4 in range(4):
                    pb = ptr[trc % 2]
                    trc += 1
                    for j in range(4):
                        kc = k4 * 4 + j
                        kb.op("pe", lambda e, pb=pb, j=j, kc=kc, x1_=x1_: e.transpose(
                            out=pb[:, j, :], in_=x1_[:, kc * 128:(kc + 1) * 128], identity=ident[:]),
                            reads=[x1_, ident], writes=[pb], wadd=(j > 0))
                    kb.op("act", lambda e, pb=pb, k4=k4: e.activation(out=xf[:, k4 * 4:(k4 + 1) * 4, :], in_=pb[:], func=AF.Copy),
                          reads=[pb], writes=[xf], wadd=(k4 > 0))
                    kb.op("dve", lambda e, k4=k4, tt=tt: e.tensor_copy(
                        out=x1Tb[:, k4 * 4:(k4 + 1) * 4, tt * 128:(tt + 1) * 128], in_=xf[:, k4 * 4:(k4 + 1) * 4, :]),
                        reads=[xf], writes=[x1Tb], wadd=not (tt == 0 and k4 == 0))
                for kc in range(16):
                    kb.op("pe", lambda e, kc=kc: e.matmul(prt[:, 0:16], lhsT=xf[:, kc, :], rhs=wr[:, kc, :], start=(kc == 0), stop=(kc == 15)),
                          reads=[xf, wr], writes=[prt], wadd=(kc > 0))
                gt_ = gate[tt % 2]
                route(kb, prt, brt, R, gt_)
                kb.dma("pool", gates[tok0:tok0 + 128, :], gt_[:], reads=[gt_])
                ti = tbk * 4 + tt
                kb.op("act", lambda e: e.activation(out=chb[:], in_=R["ch"][:], func=AF.Copy), reads=[R["ch"]], writes=[chb])
                kb.op("pe", lambda e: e.matmul(prt[:, 32:48], lhsT=lo[:, 0:128], rhs=chb[:], start=True, stop=True), reads=[lo, chb], writes=[prt])
                kb.op("pe", lambda e: e.matmul(prt[:, 48:64], lhsT=lo[:, 128:256], rhs=chb[:], start=True, stop=True), reads=[lo, chb], writes=[prt], wadd=True)
                if ti == 0:
                    kb.op("dve", lambda e: e.tensor_copy(out=R["pos"][:], in_=prt[:, 32:48]), reads=[prt], writes=[R["pos"]])
                    kb.op("dve", lambda e: e.tensor_copy(out=R["cntb"][:], in_=prt[:, 48:64]), reads=[prt], writes=[R["cntb"]])
                else:
                    kb.op("dve", lambda e: e.tensor_tensor(out=R["pos"][:], in0=prt[:, 32:48], in1=R["cntb"][:], op=ALU.add),
                          reads=[prt, R["cntb"]], writes=[R["pos"]])
                    kb.op("dve", lambda e: e.tensor_tensor(out=R["cntb"][:], in0=prt[:, 48:64], in1=R["cntb"][:], op=ALU.add),
                          reads=[prt, R["cntb"]], writes=[R["cntb"]])
                kb.op("dve", lambda e: e.tensor_scalar(out=R["ovf"][:], in0=R["pos"][:], scalar1=float(CAP), scalar2=1e7, op0=ALU.is_ge, op1=ALU.mult),
                      reads=[R["pos"]], writes=[R["ovf"]])
                kb.op("dve", lambda e: e.tensor_tensor(out=R["pos"][:], in0=R["pos"][:], in1=rowb[:], op=ALU.add), reads=[R["pos"], rowb], writes=[R["pos"]])
                kb.op("dve", lambda e: e.tensor_tensor(out=R["pos"][:], in0=R["pos"][:], in1=R["ovf"][:], op=ALU.add), reads=[R["pos"], R["ovf"]], writes=[R["pos"]])
                g_ = g12[tt % 2]
                ri_ = r12i[tt % 2]
                for c_, sel_ in ((0, "is1"), (1, "is2")):
                    kb.op("dve", lambda e, sel_=sel_: e.tensor_tensor(out=R["tmp"][:], in0=R[sel_][:], in1=R["pos"][:], op=ALU.mult),
                          reads=[R[sel_], R["pos"]], writes=[R["tmp"]])
                    kb.op("dve", lambda e, c_=c_: e.tensor_reduce(out=R["r12"][:, c_:c_ + 1], in_=R["tmp"][:], axis=AX.X, op=ALU.add),
                          reads=[R["tmp"]], writes=[R["r12"]], wadd=(c_ > 0))
                    kb.op("dve", lambda e, sel_=sel_, gt_=gt_: e.tensor_tensor(out=R["tmp"][:], in0=R[sel_][:], in1=gt_[:], op=ALU.mult),
                          reads=[R[sel_], gt_], writes=[R["tmp"]])
                    kb.op("dve", lambda e, c_=c_, g_=g_: e.tensor_reduce(out=g_[:, c_:c_ + 1], in_=R["tmp"][:], axis=AX.X, op=ALU.add),
                          reads=[R["tmp"]], writes=[g_], wadd=(c_ > 0))
                kb.op("dve", lambda e, ri_=ri_: e.tensor_copy(out=ri_[:], in_=R["r12"][:]), reads=[R["r12"]], writes=[ri_])
                tk_ = tk1[tt % 2]
                kb.op("dve", lambda e, tk_=tk_, ti=ti: e.tensor_copy(out=tk_[:], in_=tki[:, ti:ti + 1]), reads=[tki], writes=[tk_])
                for c_ in range(2):
                    rc_ = r1c[(tt % 2) * 2 + c_]
                    kb.op("dve", lambda e, rc_=rc_, c_=c_: e.tensor_copy(out=rc_[:], in_=R["r12"][:, c_:c_ + 1]), reads=[R["r12"]], writes=[rc_])
                    kb.idma(lst, tk_[:], rc_[:], True, 16 * CAP - 1, reads=[tk_, rc_, dl], writes=[dl], wadd=True)
                kb.dma("pool", ridx_d[tok0:tok0 + 128, :], ri_[:], reads=[ri_])
                kb.dma("pool", gsel_d[tok0:tok0 + 128, :], g_[:], reads=[g_])
                xb_ = x1b[tt % 2]
                kb.op("act", lambda e, xb_=xb_, x1_=x1_: e.activation(out=xb_[:], in_=x1_[:], func=AF.Copy), reads=[x1_], writes=[xb_])
                kb.dma("pool", x1b_d[tok0:tok0 + 128, :], xb_[:], reads=[xb_])
            for tt in range(4):
                phase3(tt)
            kb.dma("pool", x1T[:, tsl].rearrange("(k p) t -> p k t", p=128), x1Tb[:], reads=[x1Tb])
        kb.flush()


def route(kb, prt, brt, R, gate):
    def v3(b):
        return b[:].rearrange("p (g e) -> p g e", g=4)

    def bc(b):
        return b[:].unsqueeze(2).broadcast_to([128, 4, 4])
    D_ = "dve"
    kb.op("act", lambda e: e.activation(out=R["aff"][:], in_=prt[:, 0:16], func=AF.Sigmoid), reads=[prt], writes=[R["aff"]])
    kb.op(D_, lambda e: e.tensor_tensor(out=R["sel"][:], in0=R["aff"][:], in1=brt[:], op=ALU.add), reads=[R["aff"], brt], writes=[R["sel"]])
    kb.op(D_, lambda e: e.tensor_reduce(out=R["m1"][:], in_=v3(R["sel"]), axis=AX.X, op=ALU.max), reads=[R["sel"]], writes=[R["m1"]])
    kb.op(D_, lambda e: e.tensor_tensor(out=v3(R["eq"]), in0=v3(R["sel"]), in1=bc(R["m1"]), op=ALU.is_equal),
          reads=[R["sel"], R["m1"]], writes=[R["eq"]])
    kb.op(D_, lambda e: e.scalar_tensor_tensor(out=R["sel2"][:], in0=R["eq"][:], scalar=-1e9, in1=R["sel"][:], op0=ALU.mult, op1=ALU.add),
          reads=[R["eq"], R["sel"]], writes=[R["sel2"]])
    kb.op(D_, lambda e: e.tensor_reduce(out=R["m2"][:], in_=v3(R["sel2"]), axis=AX.X, op=ALU.max), reads=[R["sel2"]], writes=[R["m2"]])
    kb.op(D_, lambda e: e.tensor_tensor(out=R["m1"][:], in0=R["m1"][:], in1=R["m2"][:], op=ALU.add), reads=[R["m1"], R["m2"]], writes=[R["m1"]])
    kb.op(D_, lambda e: e.tensor_reduce(out=R["gm"][:], in_=R["m1"][:], axis=AX.X, op=ALU.max), reads=[R["m1"]], writes=[R["gm"]])
    kb.op(D_, lambda e: e.tensor_scalar(out=R["m2"][:], in0=R["m1"][:], scalar1=R["gm"][:, 0:1], scalar2=None, op0=ALU.is_equal),
          reads=[R["m1"], R["gm"]], writes=[R["m2"]])
    kb.op(D_, lambda e: e.tensor_scalar(out=R["m2"][:], in0=R["m2"][:], scalar1=1e9, scalar2=-1e9, op0=ALU.mult, op1=ALU.add),
          reads=[R["m2"]], writes=[R["m2"]])
    kb.op(D_, lambda e: e.tensor_tensor(out=v3(R["sel2"]), in0=v3(R["sel"]), in1=bc(R["m2"]), op=ALU.add),
          reads=[R["sel"], R["m2"]], writes=[R["sel2"]])
    kb.op(D_, lambda e: e.tensor_reduce(out=R["t1"][:], in_=R["sel2"][:], axis=AX.X, op=ALU.max), reads=[R["sel2"]], writes=[R["t1"]])
    kb.op(D_, lambda e: e.tensor_scalar(out=R["is1"][:], in0=R["sel2"][:], scalar1=R["t1"][:, 0:1], scalar2=None, op0=ALU.is_equal),
          reads=[R["sel2"], R["t1"]], writes=[R["is1"]])
    kb.op(D_, lambda e: e.scalar_tensor_tensor(out=R["sel2"][:], in0=R["is1"][:], scalar=-3e9, in1=R["sel2"][:], op0=ALU.mult, op1=ALU.add),
          reads=[R["is1"], R["sel2"]], writes=[R["sel2"]])
    kb.op(D_, lambda e: e.tensor_reduce(out=R["t2"][:], in_=R["sel2"][:], axis=AX.X, op=ALU.max), reads=[R["sel2"]], writes=[R["t2"]])
    kb.op(D_, lambda e: e.tensor_scalar(out=R["is2"][:], in0=R["sel2"][:], scalar1=R["t2"][:, 0:1], scalar2=None, op0=ALU.is_equal),
          reads=[R["sel2"], R["t2"]], writes=[R["is2"]])
    kb.op(D_, lambda e: e.tensor_tensor(out=R["ch"][:], in0=R["is2"][:], in1=R["is1"][:], op=ALU.add), reads=[R["is2"], R["is1"]], writes=[R["ch"]])
    kb.op(D_, lambda e: e.tensor_tensor(out=R["eq"][:], in0=R["ch"][:], in1=R["aff"][:], op=ALU.mult), reads=[R["ch"], R["aff"]], writes=[R["eq"]])
    kb.op(D_, lambda e: e.tensor_reduce(out=R["den"][:], in_=R["eq"][:], axis=AX.X, op=ALU.add), reads=[R["eq"]], writes=[R["den"]])
    kb.op(D_, lambda e: e.reciprocal(out=R["den"][:], in_=R["den"][:]), reads=[R["den"]], writes=[R["den"]])
    kb.op(D_, lambda e: e.tensor_scalar(out=gate[:], in0=R["eq"][:], scalar1=R["den"][:, 0:1], scalar2=None, op0=ALU.mult),
          reads=[R["eq"], R["den"]], writes=[gate])


def st_moe(kb, wb, spl, cf32, cbf, xcur1, x1T, pin, xout, lst, ridx_d, gsel_d, x1b_d, ybuf):
    NBLK = CAP // 512
    with ExitStack() as st:
        ident = kb.sb(st, "ident", [128, 128], F32)
        kb.dma("sp", ident[:], cf32[:, CF_ID:CF_ID + 128], writes=[ident])
        identb = kb.sb(st, "identb", [128, 128], BF16)
        kb.dma("sp", identb[:], cbf[:, CB_ID:CB_ID + 128], writes=[identb])
        lnp = kb.sb(st, "lnp", [128, 2, 2048], F32)
        kb.dma("sp", lnp[:, 0, :], spl[:, SP["LNG"][0] + 2048:SP["LNG"][0] + 4096], writes=[lnp])
        kb.dma("sp", lnp[:, 1, :], spl[:, SP["LNB"][0] + 2048:SP["LNB"][0] + 4096], writes=[lnp], wadd=True)
        x1Tbs = [kb.sb(st, "x1Tb%d" % i, [128, 16, 512], BF16) for i in range(2)]
        acc = kb.sb(st, "acc", [128, 4, 2048], F32)
        hT = kb.sb(st, "hT", [128, 8, 512], BF16)
        wsl = [kb.sb(st, "wsl%d" % i, [128, 8192], BF16) for i in range(3)]
        wpl = [kb.sb(st, "wpl%d" % i, [128, 2, 512], BF16) for i in range(2)]
        sg = [kb.sb(st, "sg%d" % i, [128, 512], F32) for i in range(2)]
        pts = [kb.sb(st, "pt_%d" % i, [128, 4, 256], F32) for i in range(2)]
        pT = kb.sb(st, "pT", [128, 2, 512], BF16)
        x1t = [kb.sb(st, "x1t%d" % i, [128, 2048], F32) for i in range(2)]
        xs = [kb.sb(st, "xs%d" % i, [128, 2048], BF16) for i in range(4)]
        yg = [kb.sb(st, "yg%d" % i, [128, 2048], F32) for i in range(2)]
        idxt = [kb.sb(st, "idxt%d" % i, [128, 1], I32) for i in range(4)]
        ris = [kb.sb(st, "ri%d" % i, [128, 4, 2], I32) for i in range(2)]
        ric = [kb.sb(st, "ric%d" % i, [128, 1], I32) for i in range(2)]
        gss = [kb.sb(st, "gs%d" % i, [128, 4, 2], F32) for i in range(2)]
        bs = kb.sb(st, "bs", [128, 4, 6], F32)
        mv = kb.sb(st, "mv", [128, 2], F32)
        rs = kb.sb(st, "rs", [128, 1], F32)
        pg = [kb.ps(st, "pg%d" % i, [128, 512], F32) for i in range(4)]
        py = [kb.ps(st, "py%d" % i, [128, 512], F32) for i in range(2)]
        ptr = kb.ps(st, "ptr", [128, 4, 128], F32)
        ptb = kb.ps(st, "ptb", [128, 8, 128], BF16)
        dy = Buf("ybuf", None)
        wc = 0
        pc = 0
        yc_ = 0
        sc = 0
        ev = 0
        gcnt = 0
        for i in range(4):
            kb.op("pool", lambda e, i=i: e.memset(xs[i][:], 0.0), writes=[xs[i]])
        for e_ in range(16):
            for blk in range(NBLK):
                x1Tb = x1Tbs[(e_ * NBLK + blk) % 2]
                for tt in range(4):
                    row0 = e_ * CAP + blk * 512 + tt * 128
                    it_ = idxt[gcnt % 4]
                    xs_ = xs[gcnt % 4]
                    gcnt += 1
                    kb.dma("sp", it_[:], lst[row0:row0 + 128, :], writes=[it_])
                    kb.idma(xs_[:], x1b_d, it_[:, 0:1], False, T - 1, reads=[it_], writes=[xs_], wadd=True)
                    for k8 in range(2):
                        for j in range(8):
                            kc = k8 * 8 + j
                            kb.op("pe", lambda e, j=j, kc=kc, xs_=xs_: e.transpose(
                                out=ptb[:, j, :], in_=xs_[:, kc * 128:(kc + 1) * 128], identity=identb[:]),
                                reads=[xs_, identb], writes=[ptb], wadd=(j > 0))
                        oap = x1Tb[:, k8 * 8:(k8 + 1) * 8, tt * 128:(tt + 1) * 128]
                        first = (tt == 0 and k8 == 0)
                        if ev % 2 == 0:
                            kb.op("act", lambda e, oap=oap: e.activation(out=oap, in_=ptb[:], func=AF.Copy),
                                  reads=[ptb], writes=[x1Tb], wadd=not first)
                        else:
                            kb.op("dve", lambda e, oap=oap: e.tensor_copy(out=oap, in_=ptb[:]),
                                  reads=[ptb], writes=[x1Tb], wadd=not first)
                        ev += 1
                for jc in range(8):
                    w_ = wsl[wc % 3]
                    wc += 1
                    wv = w_[:, 0:4096].rearrange("p (a k j) -> p a k j", a=2, k=16)
                    kb.dma("sp", wv, wview(wb, "GU", (e_ * 8 + jc, 128 * 2 * 16 * 128), "(p a k j) -> p a k j", p=128, a=2, k=16), writes=[w_])
                    pgg, pgu = pg[pc % 4], pg[(pc + 1) % 4]
                    pc += 2
                    for (pp, ai) in ((pgg, 0), (pgu, 1)):
                        for kc in range(16):
                            kb.op("pe", lambda e, pp=pp, ai=ai, kc=kc, wv=wv, x1Tb=x1Tb: e.matmul(
                                pp[:], lhsT=wv[:, ai, kc, :], rhs=x1Tb[:, kc, :], start=(kc == 0), stop=(kc == 15)),
                                reads=[w_, x1Tb], writes=[pp], wadd=(kc > 0))
                    s_ = sg[sc % 2]
                    sc += 1
                    kb.op("act", lambda e, s_=s_, pgg=pgg: e.activation(out=s_[:], in_=pgg[:], func=AF.Silu), reads=[pgg], writes=[s_])
                    kb.op("dve", lambda e, s_=s_, pgu=pgu, jc=jc: e.tensor_tensor(out=hT[:, jc, :], in0=s_[:], in1=pgu[:], op=ALU.mult),
                          reads=[s_, pgu], writes=[hT], wadd=(jc > 0))
                for n4 in range(4):
                    w_ = wsl[wc % 3]
                    wc += 1
                    wv = w_[:, 0:4096].rearrange("p (j n) -> p j n", j=8)
                    kb.dma("sp", wv, wview(wb, "DN", (e_ * 4 + n4, 128 * 8 * 512), "(p j n) -> p j n", p=128, j=8), writes=[w_])
                    for tt in range(4):
                        p_ = py[yc_ % 2]
                        yc_ += 1
                        for jc in range(8):
                            kb.op("pe", lambda e, p_=p_, jc=jc, tt=tt, wv=wv: e.matmul(
                                p_[:], lhsT=hT[:, jc, tt * 128:(tt + 1) * 128], rhs=wv[:, jc, :], start=(jc == 0), stop=(jc == 7)),
                                reads=[hT, w_], writes=[p_], wadd=(jc > 0))
                        asl = acc[:, tt, n4 * 512:(n4 + 1) * 512]
                        firstw = (n4 == 0 and tt == 0)
                        if ev % 2 == 0:
                            kb.op("act", lambda e, p_=p_, asl=asl: e.activation(out=asl, in_=p_[:], func=AF.Copy),
                                  reads=[p_], writes=[acc], wadd=not firstw)
                        else:
                            kb.op("dve", lambda e, p_=p_, asl=asl: e.tensor_copy(out=asl, in_=p_[:]),
                                  reads=[p_], writes=[acc], wadd=not firstw)
                        ev += 1
                r0 = e_ * CAP + blk * 512
                kb.dma("act", ybuf[r0:r0 + 512, :].rearrange("(a p) d -> p a d", p=128), acc[:], reads=[acc], writes=[dy], wadd=True, dbuf=acc)
        for tbk in range(8):
            tsl = slice(tbk * 512, (tbk + 1) * 512)
            x1Tb, ri, gs, pt_ = x1Tbs[tbk % 2], ris[tbk % 2], gss[tbk % 2], pts[tbk % 2]
            kb.dma("sp", x1Tb[:], x1T[:, tsl].rearrange("(k p) t -> p k t", p=128), writes=[x1Tb])
            kb.dma("sp", ri[:], ridx_d[tsl, :].rearrange("(a p) c -> p a c", p=128), writes=[ri])
            kb.dma("sp", gs[:], gsel_d[tsl, :].rearrange("(a p) c -> p a c", p=128), writes=[gs])
            kb.dma("sp", pt_[:], pin[tsl, :].rearrange("(a p) c -> p a c", p=128), writes=[pt_])
            for tt in range(4):
                for kc in range(2):
                    kb.op("pe", lambda e, tt=tt, kc=kc, pt_=pt_: e.transpose(out=ptr[:, kc, :], in_=pt_[:, tt, kc * 128:(kc + 1) * 128], identity=ident[:]),
                          reads=[pt_, ident], writes=[ptr], wadd=(kc > 0))
                kb.op("act", lambda e, tt=tt: e.activation(out=pT[:, :, tt * 128:(tt + 1) * 128], in_=ptr[:, 0:2, :], func=AF.Copy),
                      reads=[ptr], writes=[pT], wadd=(tt > 0))
            for n4 in range(4):
                w_ = wsl[wc % 3]
                wc += 1
                wv = w_[:].rearrange("p (k n) -> p k n", k=16)
                kb.dma("sp", wv, wview(wb, "PG", (n4, 128 * 16 * 512), "(p k n) -> p k n", p=128, k=16), writes=[w_])
                wp_ = wpl[n4 % 2]
                kb.dma("sp", wp_[:], wview(wb, "PLE", (n4, 128 * 2 * 512), "(p k n) -> p k n", p=128, k=2), writes=[wp_])
                for tt in range(4):
                    pa1, pa2 = pg[pc % 4], pg[(pc + 1) % 4]
                    pc += 2
                    for kc in range(16):
                        kb.op("pe", lambda e, pa1=pa1, kc=kc, tt=tt, wv=wv, x1Tb=x1Tb: e.matmul(
                            pa1[:], lhsT=x1Tb[:, kc, tt * 128:(tt + 1) * 128], rhs=wv[:, kc, :], start=(kc == 0), stop=(kc == 15)),
                            reads=[x1Tb, w_], writes=[pa1], wadd=(kc > 0))
                    for kc in range(2):
                        kb.op("pe", lambda e, pa2=pa2, kc=kc, tt=tt, wp_=wp_: e.matmul(
                            pa2[:], lhsT=pT[:, kc, tt * 128:(tt + 1) * 128], rhs=wp_[:, kc, :], start=(kc == 0), stop=(kc == 1)),
                            reads=[pT, wp_], writes=[pa2], wadd=(kc > 0))
                    s_ = sg[sc % 2]
                    sc += 1
                    kb.op("act", lambda e, s_=s_, pa1=pa1: e.activation(out=s_[:], in_=pa1[:], func=AF.Sigmoid), reads=[pa1], writes=[s_])
                    asl = acc[:, tt, n4 * 512:(n4 + 1) * 512]
                    kb.op("dve", lambda e, s_=s_, pa2=pa2, asl=asl: e.tensor_tensor(out=asl, in0=s_[:], in1=pa2[:], op=ALU.mult),
                          reads=[s_, pa2], writes=[acc], wadd=not (n4 == 0 and tt == 0))
            for tt in range(4):
                for c_ in range(2):
                    y_ = yg[c_]
                    ic_ = ric[c_]
                    kb.op("pool", lambda e, ic_=ic_, tt=tt, c_=c_, ri=ri: e.tensor_copy(out=ic_[:], in_=ri[:, tt, c_:c_ + 1]), reads=[ri], writes=[ic_])
                    kb.idma(y_[:], ybuf, ic_[:], False, 16 * CAP - 1, reads=[ic_, dy], writes=[y_])
                    kb.op("dve", lambda e, y_=y_, tt=tt, gs=gs, c_=c_: e.scalar_tensor_tensor(
                        out=acc[:, tt, :], in0=y_[:], scalar=gs[:, tt, c_:c_ + 1], in1=acc[:, tt, :], op0=ALU.mult, op1=ALU.add),
                        reads=[y_, gs, acc], writes=[acc], wadd=True)
            for tt in range(4):
                x_ = x1t[tt % 2]
                tok0 = tbk * 512 + tt * 128
                kb.dma("sp", x_[:], xcur1[tok0:tok0 + 128, :], writes=[x_])
                kb.op("dve", lambda e, x_=x_, tt=tt: e.scalar_tensor_tensor(
                    out=acc[:, tt, :], in0=x_[:], scalar=ALPHA, in1=acc[:, tt, :], op0=ALU.mult, op1=ALU.add),
                    reads=[x_, acc], writes=[acc], wadd=True)
                ln_tile(kb, acc, (slice(None), tt, slice(None)), lnp, x_, (slice(None), slice(None)), (bs, mv, rs))
                kb.dma("pool", xout[tok0:tok0 + 128, :], x_[:], reads=[x_])
        kb.flush()


def prep_layer_weights(inp, l):
    f32 = np.float32
    out = np.empty(NW, f32)

    def put(name, arr):
        a = np.ascontiguousarray(arr, dtype=f32).reshape(-1)
        out[OFF[name]:OFF[name] + a.size] = a

    perm = np.concatenate([np.arange(0, 7680), np.arange(7728, 13872), np.arange(7680, 7728)])
    w = inp["w_in"][l][:, perm]
    put("IN", w[:, :13824].reshape(16, 128, 108, 128).transpose(2, 1, 0, 3))
    put("INL", w[:, 13824:].reshape(16, 128, 48).transpose(1, 0, 2))
    put("BR", inp["w_branch"][l].reshape(3, 8, 128, 16, 128).transpose(3, 2, 0, 1, 4))
    put("OUT", inp["w_out"][l].reshape(16, 128, 4, 512).transpose(2, 1, 0, 3))
    put("PG", inp["w_ple_gate"][l].reshape(16, 128, 4, 512).transpose(2, 1, 0, 3))
    put("PLE", inp["w_ple"][l].reshape(2, 128, 4, 512).transpose(2, 1, 0, 3))
    put("GU", inp["w_gate_up"][l].reshape(16, 16, 128, 2, 8, 128).transpose(0, 4, 2, 3, 1, 5))
    put("DN", inp["w_down"][l].reshape(16, 8, 128, 4, 512).transpose(0, 3, 2, 1, 4))
    put("LRU", np.stack([inp["lru_wa"][l], inp["lru_wx"][l]]).transpose(2, 0, 1, 3))
    put("W1", inp["phi_w1"][l].reshape(2, 32, 64, 128).transpose(2, 0, 1, 3))
    put("W2", inp["phi_w2"][l].transpose(1, 0, 2))
    put("POS", inp["cmp_pos"][l].transpose(2, 0, 1))
    return out


def prep_small(inp, l):
    f32 = np.float32
    sp = np.zeros((128, NS), f32)

    def put(name, arr):
        o, n = SP[name]
        sp[:, o:o + n] = np.asarray(arr, f32).reshape(128, n)

    put("CW", inp["conv_a_w"][l].reshape(3, 8, 128).transpose(2, 1, 0))
    put("CB", inp["conv_a_b"][l].reshape(8, 128).T)
    put("LW", inp["lru_conv_w"][l].reshape(4, 8, 128).transpose(2, 1, 0))
    put("LB", inp["lru_conv_b"][l].reshape(8, 128).T)
    put("BA", inp["lru_ba"][l].T)
    put("BX", inp["lru_bx"][l].T)
    put("LAM", inp["lru_lam"][l].reshape(8, 128).T)
    put("B1", inp["phi_b1"][l].T)
    put("WR", inp["w_router"].reshape(16, 128, 16).transpose(1, 0, 2))
    put("BR", np.broadcast_to(inp["b_router"][None, :], (128, 16)))
    put("LNG", np.broadcast_to(inp["ln_g"][l].reshape(1, 4096), (128, 4096)))
    put("LNB", np.broadcast_to(inp["ln_b"][l].reshape(1, 4096), (128, 4096)))
    return sp


def const_tables():
    f32 = np.float32
    cb = np.zeros((128, NCB), f32)
    k = np.arange(128)[:, None]
    qq = np.arange(128)[None, :]
    cb[:, CB_ID:CB_ID + 128] = np.eye(128)
    cb[:, CB_CAUS:CB_CAUS + 128] = np.where(k > qq, NEGB, 0.0)
    cb[:, CB_TAIL:CB_TAIL + 128] = np.where(k <= qq, NEGB, 0.0)
    for b in range(32):
        E = np.zeros((128, 128), f32)
        E[2 * b, 0:64] = 1.0
        E[2 * b + 1, 64:128] = 1.0
        cb[:, CB_E + b * 128:CB_E + (b + 1) * 128] = E
    c_start = np.arange(255) * 16
    s_start = np.arange(64) * 64
    ov = np.clip(np.minimum(c_start[:, None] + 32, s_start[None, :] + 64) - np.maximum(c_start[:, None], s_start[None, :]), 0, None) / 32.0
    cm = np.zeros((256, 64), f32)
    cm[:255] = ov
    for nt in range(2):
        cb[:, CB_CM + nt * 64:CB_CM + (nt + 1) * 64] = cm[nt * 128:(nt + 1) * 128]
    for a in range(32):
        for nt in range(2):
            n = nt * 128 + np.arange(128)[:, None]
            t = a * 128 + np.arange(128)[None, :]
            vis = (n <= 254) & (16 * n + 31 <= t)
            cb[:, CB_MASK + (a * 2 + nt) * 128:CB_MASK + (a * 2 + nt + 1) * 128] = np.where(vis, 0.0, NEGB)
    cf = np.zeros((128, NCF), f32)
    cf[:, CF_ID:CF_ID + 128] = np.eye(128)
    tl = np.arange(128)[:, None]
    rel = np.arange(128)[None, :] - 63
    curoff = (tl >= 64).astype(np.int64)
    cf[:, CF_WC:CF_WC + 128] = (rel < curoff)
    cf[:, CF_WF:CF_WF + 128] = np.where(rel < curoff, 0.0, np.where(rel == curoff, 1e9, -1e9))
    cf[:, CF_RB:CF_RB + 16] = (np.arange(16) * CAP)[None, :]
    cb[:, CB_L:CB_L + 128] = (k < qq)
    cb[:, CB_ONES:CB_ONES + 128] = 1.0
    slopes = 2.0 ** (-8.0 * np.arange(1, 17) / 16)
    hi = slopes.astype(f32).astype(BF).astype(np.float64)
    lo = (slopes - hi).astype(f32).astype(BF).astype(np.float64)
    pos = np.arange(4096)
    kaug = np.zeros((5, 4, 4096), f32)
    kaug[0] = kaug[1] = (pos // 128)[None, :]
    kaug[2] = kaug[3] = (pos % 128)[None, :]
    kaug[4] = 1.0
    cpos = 16 * np.arange(256) + 31
    kaugc = np.zeros((5, 4, 256), f32)
    kaugc[0] = kaugc[1] = (cpos // 128)[None, :]
    kaugc[2] = kaugc[3] = (cpos % 128)[None, :]
    kaugc[4] = 1.0
    qaug = np.zeros((5, 16, 4096), f32)
    tref = (pos // 128) * 128 + 64
    qaug[0] = (128 * hi)[:, None]
    qaug[1] = (128 * lo)[:, None]
    qaug[2] = hi[:, None]
    qaug[3] = lo[:, None]
    qaug[4] = -(hi + lo)[:, None] * tref[None, :]
    tokid = (np.arange(32)[None, :] * 128 + np.arange(128)[:, None]).astype(np.int32)
    oob = np.full((16 * CAP, 1), 1 << 30, np.int32)
    return dict(tokid=tokid, oob=oob, cbf=cb.astype(BF), cf32=cf, kaug=kaug.astype(BF), kaugc=kaugc.astype(BF), qaug=qaug.astype(BF))


def build_nc(n_layers=DEPTH, stages=None, dbg=False):
    nc = bass.Bass("TRN2", target_bir_lowering=False)

    def din(name, shape, dt):
        return nc.dram_tensor(name, shape, dt, kind="ExternalInput").ap()

    def dscr(name, shape, dt):
        return nc.dram_tensor(name, shape, dt, kind=("ExternalOutput" if dbg else "Internal")).ap()

    x = din("x", [T, D], F32)
    pin = din("pin", [n_layers * T, 256], F32)
    wl = din("wl", [n_layers * NW if (stages is None or "cast" in stages) else 128], F32)
    sp = din("sp", [n_layers * 128, NS], F32)
    cbf = din("cbf", [128, NCB], BF16)
    cf32 = din("cf32", [128, NCF], F32)
    kaug = din("kaug", [5, 4, T], BF16)
    kaugc = din("kaugc", [5, 4, 256], BF16)
    qaug = din("qaug", [5, 16, T], BF16)
    tokid = din("tokid", [128, 32], I32)
    oob = din("oob", [16 * CAP, 1], I32)
    y = nc.dram_tensor("y", [T, D], F32, kind="ExternalOutput").ap()
    wb = [nc.dram_tensor("wb%d" % i, [s1 - s0], BF16, kind="Internal").ap() for i, (s0, s1) in enumerate(SEGS)]
    zT = dscr("zT", [NZ, T], BF16)
    yT = dscr("yT", [3072, T], BF16)
    xcur1 = dscr("xcur1", [T, D], F32)
    x1T = dscr("x1T", [D, T], BF16)
    gates = dscr("gates", [T, 16], F32)
    xmid = nc.dram_tensor("xmid", [T, D], F32, kind="Internal").ap()
    lst = nc.dram_tensor("lst", [16 * CAP, 1], I32, kind="Internal").ap()
    ridx_d = nc.dram_tensor("ridx", [T, 2], I32, kind="Internal").ap()
    gsel_d = nc.dram_tensor("gsel", [T, 2], F32, kind="Internal").ap()
    x1b_d = nc.dram_tensor("x1b", [T, D], BF16, kind="Internal").ap()
    ybuf = nc.dram_tensor("ybuf", [16 * CAP, D], F32, kind="Internal").ap()
    with ExitStack() as es:
        kb = KB(nc, es)
        for l in range(n_layers):
            xin = x if l == 0 else xmid
            xout = y if l == n_layers - 1 else xmid
            spl = sp[l * 128:(l + 1) * 128, :]

            def on(s):
                return stages is None or s in stages
            wsrc = wl[l * NW:(l + 1) * NW] if on("cast") else None
            BG = True
            if on("cast"):
                st_cast(kb, cast_jobs(wsrc, wb, [0] if BG else [0, 1, 2, 3]))
            if on("inproj"):
                st_inproj(kb, xin, wb, zT, cf32, bg_jobs=(cast_jobs(wsrc, wb, [1, 2, 3]) if (BG and on("cast")) else ()))
            if on("conv"):
                st_conv(kb, zT, yT, spl)
            if on("lru"):
                st_lru(kb, zT, yT, spl, wb)
            if on("nsa"):
                st_nsa(kb, zT, yT, wb, spl, cbf, cf32, kaug, kaugc, qaug)
            if on("mix"):
                st_mix(kb, xin, zT, yT, wb, spl, cf32, xcur1, x1T, gates, cbf, tokid, oob, lst, ridx_d, gsel_d, x1b_d)
            if on("moe"):
                st_moe(kb, wb, spl, cf32, cbf, xcur1, x1T, pin[l * T:(l + 1) * T, :], xout, lst, ridx_d, gsel_d, x1b_d, ybuf)
    return nc


_NC_CACHE = {}


def kernel(**inputs):
    inp = {k: np.asarray(v) for k, v in inputs.items()}
    B = inp["x"].shape[0]
    wl = np.concatenate([prep_layer_weights(inp, l) for l in range(DEPTH)])
    sp = np.concatenate([prep_small(inp, l) for l in range(DEPTH)], axis=0)
    ct = const_tables()
    if "nc" not in _NC_CACHE:
        _NC_CACHE["nc"] = build_nc()
    nc = _NC_CACHE["nc"]
    in_maps = []
    for b in range(B):
        m = dict(x=np.ascontiguousarray(inp["x"][b]),
                 pin=np.ascontiguousarray(inp["p"][:, b]).reshape(DEPTH * T, 256),
                 wl=wl, sp=sp)
        m.update(ct)
        in_maps.append(m)
    res = run_bass_kernel_spmd(nc, in_maps, core_ids=list(range(B)))
    return np.stack([np.asarray(r["y"], dtype=np.float32) for r in res.results], axis=0)
```
